# Optimizing a Trainium2 kernel written in Bass

```python
import jax, jax.numpy as jnp
from jax import lax
import numpy as np

D_MODEL = 4096
BATCH = 2
SEQ = 8192
DEPTH = 2

BRANCH_WIDTH = D_MODEL // 4
N_BRANCHES = 3
GMLP_CHUNK = 128
GMLP_GROUP_DIM = 128
GMLP_GROUPS = BRANCH_WIDTH // GMLP_GROUP_DIM
MOBA_HEAD_DIM = 128
MOBA_HEADS = BRANCH_WIDTH // MOBA_HEAD_DIM
MOBA_BLOCK = 256
MOBA_TOPK = 3
MOBA_Q_CHUNK = 32
SWA_HEAD_DIM = 64
SWA_Q_HEADS = BRANCH_WIDTH // SWA_HEAD_DIM
SWA_KV_HEADS = SWA_Q_HEADS // 8
SWA_WINDOW = 128
SWA_KV_WIDTH = SWA_KV_HEADS * SWA_HEAD_DIM
IN_COLS = 6 * BRANCH_WIDTH + 2 * SWA_KV_WIDTH
D_FF = 2 * D_MODEL
N_ALIBI_HEADS = SWA_Q_HEADS + MOBA_HEADS
RMS_EPS = 1e-6
LN_EPS = 1e-5

kernel_name = "hybrid_gmlp_moba_swa_macaron"


def rms_norm(x, g):
    xf = x.astype(jnp.float32)
    y = xf * lax.rsqrt(jnp.mean(xf * xf, axis=-1, keepdims=True) + RMS_EPS)
    return (y * g.astype(jnp.float32)).astype(x.dtype)


def swiglu(h, w_up, w_down):
    gate, up = jnp.split(h @ w_up, 2, axis=-1)
    return (jax.nn.silu(gate) * up) @ w_down


def alibi_slopes():
    i = jnp.arange(1, N_ALIBI_HEADS + 1, dtype=jnp.float32)
    s = jnp.exp2(-8.0 * i / N_ALIBI_HEADS)
    return s[:SWA_Q_HEADS], s[SWA_Q_HEADS:]


def chunked_gmlp(z, ln_g, ln_b, w_s, b_s):
    b, s, _ = z.shape
    z = jax.nn.gelu(z, approximate=False)
    u, v = jnp.split(z, 2, axis=-1)
    vf = v.astype(jnp.float32)
    mu = jnp.mean(vf, axis=-1, keepdims=True)
    var = jnp.mean(jnp.square(vf - mu), axis=-1, keepdims=True)
    vn = ((vf - mu) * lax.rsqrt(var + LN_EPS) * ln_g.astype(jnp.float32) + ln_b.astype(jnp.float32)).astype(v.dtype)
    nc = s // GMLP_CHUNK
    vc = vn.reshape(b, nc, GMLP_CHUNK, GMLP_GROUPS, GMLP_GROUP_DIM)
    causal = jnp.tril(jnp.ones((GMLP_CHUNK, GMLP_CHUNK), dtype=bool))
    w = jnp.where(causal[None], w_s, jnp.zeros_like(w_s))
    mixed = jnp.einsum('gts,bcsgd->bctgd', w, vc) + b_s.T[None, None, :, :, None]
    return u * mixed.reshape(b, s, BRANCH_WIDTH)


def moba_attention(q, k, v, slopes):
    b, s, h, dh = q.shape
    nb = s // MOBA_BLOCK
    topk = min(MOBA_TOPK, nb)
    scale = dh ** -0.5
    k_blk = k.reshape(b, nb, MOBA_BLOCK, h, dh)
    v_blk = v.reshape(b, nb, MOBA_BLOCK, h, dh)
    k_mean = jnp.mean(k_blk.astype(jnp.float32), axis=2)
    gate = jnp.einsum('bthd,bnhd->bhtn', q.astype(jnp.float32), k_mean)
    cur_blk = jnp.arange(s) // MOBA_BLOCK
    past = jnp.arange(nb)[None, :] < cur_blk[:, None]
    gate = jnp.where(past[None, None], gate, -jnp.inf)
    _, sel = lax.top_k(gate, topk)
    sel_valid = jnp.arange(topk)[None, :] < cur_blk[:, None]

    k_bt = k_blk.transpose(0, 3, 1, 2, 4)
    v_bt = v_blk.transpose(0, 3, 1, 2, 4)
    nq = s // MOBA_Q_CHUNK
    q_c = q.reshape(b, nq, MOBA_Q_CHUNK, h, dh).transpose(1, 0, 3, 2, 4)
    sel_c = sel.reshape(b, h, nq, MOBA_Q_CHUNK, topk).transpose(2, 0, 1, 3, 4)
    valid_c = sel_valid.reshape(nq, MOBA_Q_CHUNK, topk)
    bi = jnp.arange(b)[:, None, None, None]
    hi = jnp.arange(h)[None, :, None, None]
    blk_off = jnp.arange(MOBA_BLOCK)
    n_sel = topk * MOBA_BLOCK

    def one_chunk(args):
        qc, selc, validc, ci = args
        t = ci * MOBA_Q_CHUNK + jnp.arange(MOBA_Q_CHUNK)
        own = (ci * MOBA_Q_CHUNK) // MOBA_BLOCK
        k_sel = k_bt[bi, hi, selc]
        v_sel = v_bt[bi, hi, selc]
        s_sel = jnp.einsum('bhtd,bhtjsd->bhtjs', qc, k_sel).astype(jnp.float32) * scale
        dist_sel = (t[None, None, :, None, None] - (selc[..., None] * MOBA_BLOCK + blk_off)).astype(jnp.float32)
        s_sel = s_sel - slopes[None, :, None, None, None] * dist_sel
        s_sel = jnp.where(validc[None, None, :, :, None], s_sel, -jnp.inf)
        k_own = lax.dynamic_index_in_dim(k_bt, own, axis=2, keepdims=False)
        v_own = lax.dynamic_index_in_dim(v_bt, own, axis=2, keepdims=False)
        s_own = jnp.einsum('bhtd,bhsd->bhts', qc, k_own).astype(jnp.float32) * scale
        dist_own = t[:, None] - (own * MOBA_BLOCK + blk_off)[None, :]
        s_own = jnp.where((dist_own >= 0)[None, None],
                          s_own - slopes[None, :, None, None] * dist_own.astype(jnp.float32)[None, None],
                          -jnp.inf)
        logits = jnp.concatenate([s_sel.reshape(b, h, MOBA_Q_CHUNK, n_sel), s_own], axis=-1)
        p = jax.nn.softmax(logits, axis=-1).astype(v.dtype)
        p_sel = p[..., :n_sel].reshape(b, h, MOBA_Q_CHUNK, topk, MOBA_BLOCK)
        p_own = p[..., n_sel:]
        return (jnp.einsum('bhtjs,bhtjsd->bhtd', p_sel, v_sel)
                + jnp.einsum('bhts,bhsd->bhtd', p_own, v_own))

    out = lax.map(one_chunk, (q_c, sel_c, valid_c, jnp.arange(nq)))
    return out.transpose(1, 0, 3, 2, 4).reshape(b, s, h * dh)


def swa_sink_attention(q, k, v, sinks, slopes):
    b, s, hq, dh = q.shape
    hkv = k.shape[2]
    g = hq // hkv
    w = SWA_WINDOW
    nblk = s // w
    qb = q.reshape(b, nblk, w, hkv, g, dh)

    def band(x):
        xp = jnp.pad(x, ((0, 0), (w, 0), (0, 0), (0, 0)))
        prev = xp[:, :s].reshape(b, nblk, w, hkv, dh)
        cur = x.reshape(b, nblk, w, hkv, dh)
        return jnp.concatenate([prev, cur], axis=2)

    kb, vb = band(k), band(v)
    scores = jnp.einsum('bntkgd,bnskd->bnkgts', qb, kb).astype(jnp.float32) * (dh ** -0.5)
    t_loc = jnp.arange(w)
    s_loc = jnp.arange(2 * w)
    dist = t_loc[:, None] + w - s_loc[None, :]
    in_window = (dist >= 0) & (dist < w)
    key_pos = jnp.arange(nblk)[:, None] * w - w + s_loc[None, :]
    valid = in_window[None] & (key_pos >= 0)[:, None, :]
    slopes_kg = slopes.reshape(hkv, g)
    scores = scores - slopes_kg[None, None, :, :, None, None] * dist.astype(jnp.float32)
    scores = jnp.where(valid[None, :, None, None], scores, -jnp.inf)
    sink = jnp.broadcast_to(sinks.astype(jnp.float32).reshape(hkv, g)[None, None, :, :, None, None],
                            scores.shape[:-1] + (1,))
    p = jax.nn.softmax(jnp.concatenate([scores, sink], axis=-1), axis=-1)[..., :2 * w].astype(v.dtype)
    o = jnp.einsum('bnkgts,bnskd->bntkgd', p, vb)
    return o.reshape(b, s, hq * dh)


def mixing_sublayer(h, w_in, gmlp_ln_g, gmlp_ln_b, gmlp_w_s, gmlp_b_s, swa_sinks,
                    w_gate, b_gate, w_branch, w_out):
    b, s, _ = h.shape
    proj = h @ w_in
    bw = BRANCH_WIDTH
    z_a, qkv_b, q_c, k_c, v_c = jnp.split(
        proj, [2 * bw, 5 * bw, 6 * bw, 6 * bw + SWA_KV_WIDTH], axis=-1)
    swa_slopes, moba_slopes = alibi_slopes()
    y_a = chunked_gmlp(z_a, gmlp_ln_g, gmlp_ln_b, gmlp_w_s, gmlp_b_s)
    qkv_b = qkv_b.reshape(b, s, 3, MOBA_HEADS, MOBA_HEAD_DIM)
    y_b = moba_attention(qkv_b[:, :, 0], qkv_b[:, :, 1], qkv_b[:, :, 2], moba_slopes)
    y_c = swa_sink_attention(q_c.reshape(b, s, SWA_Q_HEADS, SWA_HEAD_DIM),
                             k_c.reshape(b, s, SWA_KV_HEADS, SWA_HEAD_DIM),
                             v_c.reshape(b, s, SWA_KV_HEADS, SWA_HEAD_DIM),
                             swa_sinks, swa_slopes)
    merged = None
    for n, y in enumerate((y_a, y_b, y_c)):
        gate = jax.nn.sigmoid(h @ w_gate[:, n * D_MODEL:(n + 1) * D_MODEL]
                              + b_gate[n * D_MODEL:(n + 1) * D_MODEL])
        term = gate * (y @ w_branch[n])
        merged = term if merged is None else merged + term
    return merged @ w_out


def setup_inputs(seed: int = 0) -> dict:
    key = jax.random.key(seed)
    ks = jax.random.split(key, 24)
    f32 = jnp.float32

    def nrm(k, shape, fan_in):
        return jax.random.normal(k, shape, f32) * (fan_in ** -0.5)

    def gain(k, shape):
        return 1.0 + 0.05 * jax.random.normal(k, shape, f32)

    L, D = DEPTH, D_MODEL
    return {
        "x": jax.random.normal(ks[0], (BATCH, SEQ, D), f32),
        "ffn1_pre_g": gain(ks[1], (L, D)),
        "ffn1_w_up": nrm(ks[2], (L, D, 2 * D_FF), D),
        "ffn1_w_down": nrm(ks[3], (L, D_FF, D), D_FF),
        "ffn1_post_g": gain(ks[4], (L, D)),
        "mix_pre_g": gain(ks[5], (L, D)),
        "w_in": nrm(ks[6], (L, D, IN_COLS), D),
        "gmlp_ln_g": gain(ks[7], (L, BRANCH_WIDTH)),
        "gmlp_ln_b": 0.02 * jax.random.normal(ks[8], (L, BRANCH_WIDTH), f32),
        "gmlp_w_s": nrm(ks[9], (L, GMLP_GROUPS, GMLP_CHUNK, GMLP_CHUNK), GMLP_CHUNK),
        "gmlp_b_s": 1.0 + 0.1 * jax.random.normal(ks[10], (L, GMLP_GROUPS, GMLP_CHUNK), f32),
        "swa_sinks": 0.5 * jax.random.normal(ks[11], (L, SWA_Q_HEADS), f32),
        "w_gate": nrm(ks[12], (L, D, N_BRANCHES * D), D),
        "b_gate": 0.01 * jax.random.normal(ks[13], (L, N_BRANCHES * D), f32),
        "w_branch": nrm(ks[14], (L, N_BRANCHES, BRANCH_WIDTH, D), BRANCH_WIDTH),
        "w_out": nrm(ks[15], (L, D, D), D),
        "mix_post_g": gain(ks[16], (L, D)),
        "ffn2_pre_g": gain(ks[17], (L, D)),
        "ffn2_w_up": nrm(ks[18], (L, D, 2 * D_FF), D),
        "ffn2_w_down": nrm(ks[19], (L, D_FF, D), D_FF),
        "ffn2_post_g": gain(ks[20], (L, D)),
    }


def reference(x, ffn1_pre_g, ffn1_w_up, ffn1_w_down, ffn1_post_g, mix_pre_g, w_in,
              gmlp_ln_g, gmlp_ln_b, gmlp_w_s, gmlp_b_s, swa_sinks, w_gate, b_gate,
              w_branch, w_out, mix_post_g, ffn2_pre_g, ffn2_w_up, ffn2_w_down, ffn2_post_g):
    for i in range(DEPTH):
        x = x + 0.5 * rms_norm(swiglu(rms_norm(x, ffn1_pre_g[i]), ffn1_w_up[i], ffn1_w_down[i]),
                               ffn1_post_g[i])
        h = rms_norm(x, mix_pre_g[i])
        m = mixing_sublayer(h, w_in[i], gmlp_ln_g[i], gmlp_ln_b[i], gmlp_w_s[i], gmlp_b_s[i],
                            swa_sinks[i], w_gate[i], b_gate[i], w_branch[i], w_out[i])
        x = x + rms_norm(m, mix_post_g[i])
        x = x + 0.5 * rms_norm(swiglu(rms_norm(x, ffn2_pre_g[i]), ffn2_w_up[i], ffn2_w_down[i]),
                               ffn2_post_g[i])
    return x
```

```python
import contextlib
import os
import numpy as np
import ml_dtypes
import concourse.bass as bass
import concourse.mybir as mybir
from concourse.bass_utils import run_bass_kernel_spmd

F32 = mybir.dt.float32
BF16 = mybir.dt.bfloat16
AF = mybir.ActivationFunctionType
ALU = mybir.AluOpType
AX = mybir.AxisListType

D = 4096
NCH = D // 128
TT = 512
TOK = 2048
DFF = 8192
BW = 1024
IN_COLS = 6400
RMS_EPS = 1e-6
LN_EPS = 1e-5
NEG = -30000.0
SEM_ROT = 28000
SAME_ENGINE_SYNC = True


class Res:
    __slots__ = ("name", "last_write", "readers", "dma_readers", "aliases", "sem", "cnt")

    def __init__(self, name):
        self.name = name
        self.last_write = None
        self.readers = {}
        self.dma_readers = []
        self.aliases = []
        self.sem = None
        self.cnt = 0


def alias(a, b):
    a.aliases.append(b)
    b.aliases.append(a)


class Op:
    __slots__ = ("eng", "fn", "deps", "tick", "token", "is_dma", "dres")

    def __init__(self, eng, fn, is_dma, dres):
        self.eng = eng
        self.fn = fn
        self.deps = []
        self.tick = False
        self.token = None
        self.is_dma = is_dma
        self.dres = dres


class Sched:
    ENGS = ("pe", "act", "dve", "pool", "sp")

    def __init__(self, sem_pool):
        self.sem_pool = list(sem_pool)
        self.ops = []
        self.by_eng = {e: [] for e in self.ENGS}
        self.pending_barrier = {e: None for e in self.ENGS}
        self.dma_since_barrier = []
        self.last_op = {e: None for e in self.ENGS}

    def op(self, eng, fn, reads=(), writes=(), dma=False):
        o = Op(eng, fn, dma, writes[0] if dma else None)
        deps = []
        for r in reads:
            if r.last_write is not None:
                deps.append(r.last_write)
            for a in r.aliases:
                if a.last_write is not None:
                    deps.append(a.last_write)
        for w in writes:
            for x in [w] + w.aliases:
                if x.last_write is not None:
                    deps.append(x.last_write)
                deps.extend(x.readers.values())
                deps.extend(x.dma_readers)
        pb = self.pending_barrier[eng]
        if pb is not None:
            deps.extend(pb)
            self.pending_barrier[eng] = None
        seen = set()
        for d in deps:
            if d is o or id(d) in seen:
                continue
            seen.add(id(d))
            if (not d.is_dma) and (not dma) and d.eng == eng:
                if eng == "pe" or not SAME_ENGINE_SYNC:
                    continue
            d.tick = True
            o.deps.append(d)
        for r in reads:
            if dma:
                r.dma_readers.append(o)
            else:
                r.readers[eng] = o
        for w in writes:
            w.last_write = o
            w.readers = {}
            w.dma_readers = []
        self.ops.append(o)
        self.by_eng[eng].append(o)
        self.last_op[eng] = o
        if dma:
            self.dma_since_barrier.append(o)
        return o

    def barrier(self):
        deps = [o for o in self.last_op.values() if o is not None and not o.is_dma]
        deps += self.dma_since_barrier
        self.dma_since_barrier = []
        for e in self.ENGS:
            old = self.pending_barrier[e] or []
            self.pending_barrier[e] = old + deps

    def number(self):
        eng_sem = {}
        eng_cnt = {}
        for o in self.ops:
            if o.is_dma:
                r = o.dres
                if r.sem is None or r.cnt >= SEM_ROT:
                    r.sem = self.sem_pool.pop()
                    r.cnt = 0
                r.cnt += 16
                o.token = (r.sem, r.cnt)
            elif o.tick:
                e = o.eng
                if e not in eng_sem or eng_cnt[e] >= SEM_ROT:
                    eng_sem[e] = self.sem_pool.pop()
                    eng_cnt[e] = 0
                eng_cnt[e] += 1
                o.token = (eng_sem[e], eng_cnt[e])

    def emit(self, eng_name, eng):
        waited = {}
        for o in self.by_eng[eng_name]:
            for d in o.deps:
                sem, val = d.token
                k = id(sem)
                if waited.get(k, (None, 0))[1] < val:
                    eng.wait_ge(sem, val)
                    waited[k] = (sem, val)
            ins = o.fn(eng)
            if o.token is not None:
                ins.then_inc(o.token[0], 16 if o.is_dma else 1)


class Builder:
    def __init__(self, nc, S, arena, ps, NT):
        self.nc = nc
        self.S = S
        self.arena = arena
        self.ps = ps
        self.NT = NT
        self.top = 0
        self.bank_res = [Res(f"bank{i}") for i in range(8)]
        self.dram = {}

    def alloc(self, nbytes, dtype, shape_free):
        assert nbytes % 4 == 0
        off = self.top // 4
        self.top += nbytes
        assert self.top <= self.arena_bytes, (self.top, self.arena_bytes)
        ap = self.arena[:, off:off + nbytes // 4]
        if dtype != F32:
            ap = ap.bitcast(dtype)
        if len(shape_free) == 2:
            ap = ap.rearrange("p (a b) -> p a b", a=shape_free[0])
        elif len(shape_free) == 3:
            ap = ap.rearrange("p (a b c) -> p a b c", a=shape_free[0], b=shape_free[1])
        return ap

    def bank(self, i, dtype=F32):
        ap = self.ps[:, i * 512:(i + 1) * 512]
        if dtype != F32:
            ap = ap.bitcast(dtype)
        return ap


def _mm(out, lhsT, rhs, start, stop):
    return lambda e: e.matmul(out, lhsT, rhs, start=start, stop=stop)


def _act(out, in_, func, bias=None, scale=None):
    kw = {}
    if bias is not None:
        kw["bias"] = bias
    if scale is not None:
        kw["scale"] = scale
    return lambda e: e.activation(out, in_, func, **kw)


def _dma(out, in_):
    return lambda e: e.dma_start(out=out, in_=in_)


def _ts(out, in0, s1, s2, op0, op1=None):
    if op1 is None:
        return lambda e: e.tensor_scalar(out, in0, s1, None, op0)
    return lambda e: e.tensor_scalar(out, in0, s1, s2, op0, op1)


def _tt(out, in0, in1, op):
    return lambda e: e.tensor_tensor(out, in0, in1, op)


def _stt(out, in0, scalar, in1, op0, op1):
    return lambda e: e.scalar_tensor_tensor(out, in0, scalar, in1, op0, op1)


def _copy(out, in_):
    return lambda e: e.tensor_copy(out, in_)


class Common:
    pass


def setup_common(B, consts_ap, gains_ap):
    S = B.S
    C = Common()
    C.ones = B.alloc(256, BF16, (128,))
    C.ident = B.alloc(256, BF16, (128,))
    C.r_const = Res("const")
    S.op("sp", _dma(C.ident, consts_ap[:, 0:128]), writes=[C.r_const], dma=True)
    S.op("dve", lambda e: e.memset(C.ones, 1.0), writes=[C.r_const])
    ng = gains_ap.shape[1]
    C.gains = B.alloc(ng * 4, F32, (ng,))
    C.r_gains = Res("gains")
    S.op("sp", _dma(C.gains, gains_ap), writes=[C.r_gains], dma=True)
    C.slots = [B.alloc(8192, BF16, (4, 1024)) for _ in range(3)]
    C.slot_res = [Res(f"slot{i}") for i in range(3)]
    C.slot_i = 0
    C.rstd = [B.alloc(2048, F32, (512,)) for _ in range(2)]
    C.r_rstd = [Res("rstd0"), Res("rstd1")]
    C.sq = [B.alloc(1024, BF16, (512,)) for _ in range(2)]
    C.r_sq = [Res("sq0"), Res("sq1")]
    C.tmp = [B.alloc(2048, F32, (512,)) for _ in range(4)]
    C.r_tmp = [Res(f"tmp{i}") for i in range(4)]
    C.tmp_i = 0
    return C


def next_tmp(C):
    i = C.tmp_i
    C.tmp_i = (i + 1) % 4
    return C.tmp[i], C.r_tmp[i]


def stream_weights(B, C, w_ap, row0, nk, col_ranges, consume):
    S = B.S
    nq = (nk + 3) // 4
    for q in range(nq):
        si = C.slot_i
        C.slot_i = (si + 1) % 3
        slot = C.slots[si]
        sres = C.slot_res[si]
        kc_n = min(4, nk - q * 4)
        off = 0
        for (c0, n) in col_ranges:
            src = w_ap[row0 + q * 512: row0 + q * 512 + kc_n * 128, c0:c0 + n].rearrange(
                "(kc p) c -> p kc c", p=128)
            S.op("pool", _dma(slot[:, 0:kc_n, off:off + n], src), writes=[sres], dma=True)
            off += n
        for kc in range(kc_n):
            k = q * 4 + kc
            consume(k, (lambda o, n, _s=slot, _kc=kc: _s[:, _kc, o:o + n]), sres)


def rms_stats(B, C, chunks, chunk_res, which, eps, bank_i):
    S = B.S
    bank = B.bank(bank_i)
    bres = B.bank_res[bank_i]
    n = len(chunks)
    for c in range(n):
        sq, rsq = C.sq[c % 2], C.r_sq[c % 2]
        S.op("act", _act(sq, chunks[c], AF.Square), reads=[chunk_res[c]], writes=[rsq])
        S.op("pe", _mm(bank, C.ones, sq, c == 0, c == n - 1), reads=[rsq, C.r_const], writes=[bres])
    rstd, rr = C.rstd[which], C.r_rstd[which]
    S.op("dve", _ts(rstd, bank, 1.0 / D, eps, ALU.mult, ALU.add), reads=[bres], writes=[rr])
    S.op("act", _act(rstd, rstd, AF.Sqrt), reads=[rr], writes=[rr])
    S.op("dve", lambda e: e.reciprocal(rstd, rstd), reads=[rr], writes=[rr])
    return rstd, rr


def load_x_tile(B, C, X, r_X, x_in, t):
    S = B.S
    for q in range(4):
        src = x_in[q * 1024:(q + 1) * 1024, t * TT:(t + 1) * TT].rearrange("(c p) t -> p c t", p=128)
        S.op("sp", _dma(X[:, q * 8:(q + 1) * 8, :], src), reads=[B.dram_res(x_in)], writes=[r_X[q]], dma=True)


def make_hn(B, C, X, r_X, HN, r_HN, gcol):
    S = B.S
    rstd, rr = rms_stats(B, C, [X[:, c, :] for c in range(NCH)], [r_X[c // 8] for c in range(NCH)], 0, RMS_EPS, 0)
    for c in range(NCH):
        S.op("dve", _stt(HN[:, c, :], X[:, c, :], C.gains[:, gcol + c:gcol + c + 1], rstd, ALU.mult, ALU.mult),
             reads=[r_X[c // 8], rr, C.r_gains], writes=[r_HN[c]])


def residual_tail(B, C, t, x_in, x_out, yscr, gcol, half):
    S = B.S
    rstd, rr = C.rstd[1], C.r_rstd[1]
    for c in range(NCH):
        ty, rty = next_tmp(C)
        tx, rtx = next_tmp(C)
        S.op("sp", _dma(ty, yscr[c * 128:(c + 1) * 128, :]), reads=[B.dram_res(yscr)], writes=[rty], dma=True)
        S.op("sp", _dma(tx, x_in[c * 128:(c + 1) * 128, t * TT:(t + 1) * TT]), reads=[B.dram_res(x_in)],
             writes=[rtx], dma=True)
        S.op("dve", _stt(ty, ty, C.gains[:, gcol + c:gcol + c + 1], rstd, ALU.mult, ALU.mult),
             reads=[rty, rr, C.r_gains], writes=[rty])
        if half:
            S.op("dve", _stt(tx, ty, 0.5, tx, ALU.mult, ALU.add), reads=[rty, rtx], writes=[rtx])
        else:
            S.op("dve", _tt(tx, ty, tx, ALU.add), reads=[rty, rtx], writes=[rtx])
        S.op("sp", _dma(x_out[c * 128:(c + 1) * 128, t * TT:(t + 1) * TT], tx), reads=[rtx],
             writes=[B.dram_res(x_out)], dma=True)


def out_groups(B, C, w_ap, row0, rhs_chunks, rhs_res, yscr, evac_extra=None):
    S = B.S
    nk = len(rhs_chunks)
    sbank, sres = B.bank(7), B.bank_res[7]
    groups = [list(range(i, min(i + 7, NCH))) for i in range(0, NCH, 7)]
    done = 0
    for grp in groups:
        c0 = grp[0] * 128
        ncols = len(grp) * 128

        def consume(k, view, sres_slot, grp=grp):
            for j, m in enumerate(grp):
                S.op("pe", _mm(B.bank(j), view(j * 128, 128), rhs_chunks[k], k == 0, k == nk - 1),
                     reads=[sres_slot, rhs_res[k]], writes=[B.bank_res[j]])
        stream_weights(B, C, w_ap, row0, nk, [(c0, ncols)], consume)
        for j, m in enumerate(grp):
            ty, rty = next_tmp(C)
            eng = "act" if (j % 2 == 0) else "dve"
            if eng == "act":
                S.op("act", _act(ty, B.bank(j), AF.Copy), reads=[B.bank_res[j]], writes=[rty])
            else:
                S.op("dve", _copy(ty, B.bank(j)), reads=[B.bank_res[j]], writes=[rty])
            sq, rsq = C.sq[done % 2], C.r_sq[done % 2]
            S.op("act", _act(sq, ty, AF.Square), reads=[rty], writes=[rsq])
            S.op("pe", _mm(sbank, C.ones, sq, done == 0, done == NCH - 1), reads=[rsq, C.r_const], writes=[sres])
            S.op("sp", _dma(yscr[m * 128:(m + 1) * 128, :], ty), reads=[rty], writes=[B.dram_res(yscr)], dma=True)
            done += 1
    rstd, rr = C.rstd[1], C.r_rstd[1]
    S.op("dve", _ts(rstd, sbank, 1.0 / D, RMS_EPS, ALU.mult, ALU.add), reads=[sres], writes=[rr])
    S.op("act", _act(rstd, rstd, AF.Sqrt), reads=[rr], writes=[rr])
    S.op("dve", lambda e: e.reciprocal(rstd, rstd), reads=[rr], writes=[rr])


def ffn_stage(B, C, x_in, x_out, yscr, w_up, w_down, g_pre_col, g_post_col):
    S = B.S
    S.barrier()
    base = B.top
    X = B.alloc(65536, F32, (32, 512))
    r_X = [Res(f"X{q}") for q in range(4)]
    HN = B.alloc(32768, BF16, (32, 512))
    r_HN = [Res(f"HN{c}") for c in range(NCH)]
    ACTB = X.rearrange("p a b -> p (a b)").bitcast(BF16).rearrange("p (a b) -> p a b", a=64)
    r_A = [Res(f"A{j}") for j in range(64)]
    for j in range(64):
        alias(r_A[j], r_X[j // 16])
    for t in range(B.NT):
        load_x_tile(B, C, X, r_X, x_in, t)
        make_hn(B, C, X, r_X, HN, r_HN, g_pre_col)
        for g in range(16):
            def consume(k, view, sres_slot):
                for j in range(8):
                    S.op("pe", _mm(B.bank(j), view(j * 128, 128), HN[:, k, :], k == 0, k == NCH - 1),
                         reads=[sres_slot, r_HN[k]], writes=[B.bank_res[j]])
            stream_weights(B, C, w_up, 0, NCH, [(g * 512, 512), (DFF + g * 512, 512)], consume)
            for j in range(4):
                ty, rty = next_tmp(C)
                S.op("act", _act(ty, B.bank(j), AF.Silu), reads=[B.bank_res[j]], writes=[rty])
                S.op("dve", _tt(ACTB[:, g * 4 + j, :], ty, B.bank(4 + j), ALU.mult),
                     reads=[rty, B.bank_res[4 + j]], writes=[r_A[g * 4 + j]])
        out_groups(B, C, w_down, 0, [ACTB[:, k, :] for k in range(64)], r_A, yscr)
        residual_tail(B, C, t, x_in, x_out, yscr, g_post_col, True)
    B.top = base


def _builder_extras():
    def dram_res(self, ap):
        name = ap.tensor.name if hasattr(ap, "tensor") else ap.name
        if name not in self.dram:
            self.dram[name] = Res("dram_" + name)
        return self.dram[name]
    Builder.dram_res = dram_res


_builder_extras()

ARENA_BYTES = 204 * 1024


def make_program(stage_fn, NT=4):
    nc = bass.Bass("TRN2", target_bir_lowering=False)
    with contextlib.ExitStack() as es:
        arena = es.enter_context(nc.sbuf_tensor("arena", [128, ARENA_BYTES // 4], F32))
        ps = es.enter_context(nc.psum_tensor("ps", [128, 4096], F32))
        sems = [es.enter_context(nc.semaphore(f"s{i}")) for i in range(90)]
        S = Sched(sems)
        B = Builder(nc, S, arena, ps, NT)
        B.arena_bytes = ARENA_BYTES
        stage_fn(nc, B)
        S.number()
        block = es.enter_context(nc.Block())

        @block.tensor
        def _(e):
            S.emit("pe", e)

        @block.scalar
        def _(e):
            S.emit("act", e)

        @block.vector
        def _(e):
            S.emit("dve", e)

        @block.gpsimd
        def _(e):
            S.emit("pool", e)

        @block.sync
        def _(e):
            S.emit("sp", e)
    return nc


def final_wait(B, out_res_list):
    B.S.op("sp", lambda e: None, reads=out_res_list)


def gcol(l, i):
    return (l * 6 + i) * 32


BG0 = 2 * 6 * 32
SINK0 = BG0 + 2 * 96
NG = SINK0 + 16

BC_IDENT = 0
BC_TRI = 128
BC_EALL = 256
BC_CB = BC_EALL + 4096
BC_ONEA = BC_CB + 2048
BC_ONEB = BC_ONEA + 128
NBC = BC_ONEB + 128
FC_ALB = 0
FC_PASTB = FC_ALB + 8 * 4 * 68
FC_SWAB = FC_PASTB + 512
FC_HALOW = FC_SWAB + 8 * 2 * 512
NFC = FC_HALOW + 4


def fm_group(B, C, w_ap, col0, nchunks, HN, r_HN, evac):
    S = B.S

    def consume(k, view, sres_slot):
        for j in range(nchunks):
            S.op("pe", _mm(B.bank(j), view(j * 128, 128), HN[:, k, :], k == 0, k == NCH - 1),
                 reads=[sres_slot, r_HN[k]], writes=[B.bank_res[j]])
    nc_ = nchunks * 128
    rngs = [(col0, nc_)] if nc_ <= 512 else [(col0, 512), (col0 + 512, nc_ - 512)]
    stream_weights(B, C, w_ap, 0, NCH, rngs, consume)
    for j in range(nchunks):
        evac(j)


def tm_group(B, C, w_ap, col0, HN, r_HN):
    S = B.S

    def consume(k, view, sres_slot):
        for s in range(4):
            for cb in range(2):
                S.op("pe", _mm(B.bank(s * 2 + cb), HN[:, k, s * 128:(s + 1) * 128], view(cb * 512, 512),
                               k == 0, k == NCH - 1),
                     reads=[sres_slot, r_HN[k]], writes=[B.bank_res[s * 2 + cb]])
    stream_weights(B, C, w_ap, 0, NCH, [(col0, 512), (col0 + 512, 512)], consume)


def mixA_stage(B, C, l, x_in, w_in, wsT, rowtab, bconst, sc):
    S = B.S
    S.barrier()
    base = B.top
    X = B.alloc(65536, F32, (32, 512))
    r_X = [Res(f"X{q}") for q in range(4)]
    HN = B.alloc(32768, BF16, (32, 512))
    r_HN = [Res(f"HN{c}") for c in range(NCH)]
    ROW = B.alloc(12288, F32, (3, 1024))
    r_ROW = Res("rowtab")
    WSF = B.alloc(4096, F32, (8, 128))
    WST = B.alloc(2048, BF16, (8, 128))
    TRI = B.alloc(256, BF16, (128,))
    r_WS = Res("wst")
    LNS = B.alloc(64, F32, (16,))
    r_LNS = Res("lns")
    KM = B.alloc(256, F32, (8, 8))
    r_KM = Res("km")
    xb = X.rearrange("p a b -> p (a b)")

    def sub(off, nbytes, dtype, shape):
        ap = xb[:, off // 4:(off + nbytes) // 4]
        if dtype != F32:
            ap = ap.bitcast(dtype)
        if len(shape) == 2:
            ap = ap.rearrange("p (a b) -> p a b", a=shape[0])
        return ap

    def xres(name):
        r = Res(name)
        for q in range(4):
            alias(r, r_X[q])
        return r
    U = sub(0, 8192, BF16, (8, 512)); r_U = xres("U")
    STG = [sub(8192, 8192, BF16, (8, 512)), sub(16384, 8192, BF16, (8, 512))]
    r_STG = [xres("STG0"), xres("STG1")]
    VG = [sub(24576, 4096, F32, (1024,)), sub(28672, 4096, F32, (1024,))]
    r_VG = [xres("VG0"), xres("VG1")]
    VN = [sub(32768, 2048, BF16, (1024,)), sub(34816, 2048, BF16, (1024,))]
    r_VN = [xres("VN0"), xres("VN1")]
    YAST = sub(36864, 8192, BF16, (8, 512)); r_YAST = xres("YAST")
    VBST = [sub(45056, 2048, BF16, (1024,)), sub(47104, 2048, BF16, (1024,))]
    r_VBST = [xres("VBST0"), xres("VBST1")]
    VCST = sub(49152, 1024, BF16, (4, 128)); r_VCST = xres("VCST")
    stg_i = [0]

    S.op("sp", _dma(ROW, rowtab), writes=[r_ROW], dma=True)
    S.op("sp", _dma(WSF, wsT), writes=[r_WS], dma=True)
    S.op("sp", _dma(TRI, bconst[:, BC_TRI:BC_TRI + 128]), writes=[r_WS], dma=True)
    for g in range(8):
        S.op("dve", _tt(WST[:, g, :], WSF[:, g, :], TRI, ALU.mult), reads=[r_WS], writes=[r_WS])
    qscale = 128.0 ** -0.5

    for t in range(B.NT):
        tsl = slice(t * TT, (t + 1) * TT)
        load_x_tile(B, C, X, r_X, x_in, t)
        make_hn(B, C, X, r_X, HN, r_HN, gcol(l, 2))
        SECT = os.environ.get("MIXA_SECT", "u,v,qb,kb,vb,qc,kcvc").split(",")
        if "u" in SECT:
          fm_group(B, C, w_in, 0, 8, HN, r_HN,
                 lambda j: S.op("act", _act(U[:, j, :], B.bank(j), AF.Gelu), reads=[B.bank_res[j]], writes=[r_U]))
        if "v" in SECT:
            tm_group(B, C, w_in, 1024, HN, r_HN)
        for s in (range(4) if "v" in SECT else []):
            vg, rvg = VG[s % 2], r_VG[s % 2]
            vn, rvn = VN[s % 2], r_VN[s % 2]
            for cb in range(2):
                S.op("act", _act(vg[:, cb * 512:(cb + 1) * 512], B.bank(s * 2 + cb), AF.Gelu),
                     reads=[B.bank_res[s * 2 + cb]], writes=[rvg])
            for cb in range(2):
                S.op("dve", (lambda e, cb=cb, vg=vg: e.bn_stats(LNS[:, cb * 6:(cb + 1) * 6], vg[:, cb * 512:(cb + 1) * 512])),
                     reads=[rvg], writes=[r_LNS])
            S.op("dve", lambda e: e.bn_aggr(LNS[:, 12:14], LNS[:, 0:12]), reads=[r_LNS], writes=[r_LNS])
            S.op("dve", _ts(LNS[:, 13:14], LNS[:, 13:14], LN_EPS, None, ALU.add), reads=[r_LNS], writes=[r_LNS])
            S.op("act", _act(LNS[:, 13:14], LNS[:, 13:14], AF.Sqrt), reads=[r_LNS], writes=[r_LNS])
            S.op("dve", lambda e: e.reciprocal(LNS[:, 13:14], LNS[:, 13:14]), reads=[r_LNS], writes=[r_LNS])
            S.op("dve", _ts(vg, vg, LNS[:, 12:13], LNS[:, 13:14], ALU.subtract, ALU.mult), reads=[rvg, r_LNS], writes=[rvg])
            S.op("dve", _tt(vg, vg, ROW[:, 1, :], ALU.mult), reads=[rvg, r_ROW], writes=[rvg])
            S.op("dve", _tt(vn, vg, ROW[:, 2, :], ALU.add), reads=[rvg, r_ROW], writes=[rvn])
            for g in range(8):
                bk = s * 2 + g // 4
                S.op("pe", _mm(B.bank(bk)[:, (g % 4) * 128:(g % 4 + 1) * 128], vn[:, g * 128:(g + 1) * 128],
                               WST[:, g, :], True, True),
                     reads=[rvn, r_WS], writes=[B.bank_res[bk]])
            for gh in range(2):
                bk = s * 2 + gh
                tm, rtm = next_tmp(C)
                S.op("dve", _tt(tm, B.bank(bk), ROW[:, 0, gh * 512:(gh + 1) * 512], ALU.add),
                     reads=[B.bank_res[bk], r_ROW], writes=[rtm])
                S.op("dve", _tt(YAST[:, gh * 4:(gh + 1) * 4, s * 128:(s + 1) * 128],
                                tm.rearrange("p (a b) -> p a b", a=4),
                                U[:, gh * 4:(gh + 1) * 4, s * 128:(s + 1) * 128], ALU.mult),
                     reads=[rtm, r_U], writes=[r_YAST])
        S.op("sp", _dma(sc["yaT"][:, tsl].rearrange("(g p) t -> p g t", p=128), YAST), reads=[r_YAST],
             writes=[B.dram_res(sc["yaT"])], dma=True)

        def fm_to_scratch(col0, dst, scale, with_km=False):
            i = stg_i[0]; stg_i[0] = 1 - i
            stg, rstg = STG[i], r_STG[i]

            def ev(j):
                if with_km:
                    for hb in range(2):
                        S.op("act", (lambda e, j=j, t=t, hb=hb: e.activation(
                            stg[:, j, hb * 256:(hb + 1) * 256], B.bank(j)[:, hb * 256:(hb + 1) * 256], AF.Copy,
                            accum_out=KM[:, j, t * 2 + hb:t * 2 + hb + 1])),
                             reads=[B.bank_res[j]], writes=[rstg, r_KM])
                elif scale is not None:
                    S.op("dve", _ts(stg[:, j, :], B.bank(j), scale, None, ALU.mult), reads=[B.bank_res[j]], writes=[rstg])
                else:
                    S.op("act", _act(stg[:, j, :], B.bank(j), AF.Copy), reads=[B.bank_res[j]], writes=[rstg])
            fm_group(B, C, w_in, col0, 8, HN, r_HN, ev)
            S.op("sp", _dma(dst[:, tsl].rearrange("(g p) t -> p g t", p=128), stg), reads=[rstg],
                 writes=[B.dram_res(dst)], dma=True)
        if "qb" in SECT:
            fm_to_scratch(2048, sc["qbT"], qscale)
        if "kb" in SECT:
            fm_to_scratch(3072, sc["kbT"], None, with_km=True)
        if "vb" in SECT:
            tm_group(B, C, w_in, 4096, HN, r_HN)
        for s in (range(4) if "vb" in SECT else []):
            vb, rvb = VBST[s % 2], r_VBST[s % 2]
            S.op("act", _act(vb[:, 0:512], B.bank(s * 2), AF.Copy), reads=[B.bank_res[s * 2]], writes=[rvb])
            S.op("dve", _copy(vb[:, 512:1024], B.bank(s * 2 + 1)), reads=[B.bank_res[s * 2 + 1]], writes=[rvb])
            S.op("sp", _dma(sc["vb"][t * TT + s * 128:t * TT + (s + 1) * 128, :], vb), reads=[rvb],
                 writes=[B.dram_res(sc["vb"])], dma=True)
        if "qc" in SECT:
            fm_to_scratch(5120, sc["qcT"], 0.125)
        if "kcvc" not in SECT:
            continue
        i = stg_i[0]; stg_i[0] = 1 - i
        stg, rstg = STG[i], r_STG[i]

        def consume(k, view, sres_slot):
            S.op("pe", _mm(B.bank(0), view(0, 128), HN[:, k, :], k == 0, k == NCH - 1),
                 reads=[sres_slot, r_HN[k]], writes=[B.bank_res[0]])
            for s in range(4):
                S.op("pe", _mm(B.bank(1 + s)[:, 0:128], HN[:, k, s * 128:(s + 1) * 128], view(128, 128),
                               k == 0, k == NCH - 1),
                     reads=[sres_slot, r_HN[k]], writes=[B.bank_res[1 + s]])
        stream_weights(B, C, w_in, 0, NCH, [(6144, 256)], consume)
        S.op("act", _act(stg[:, 0, :], B.bank(0), AF.Copy), reads=[B.bank_res[0]], writes=[rstg])
        S.op("sp", _dma(sc["kcT"][:, tsl], stg[:, 0, :]), reads=[rstg], writes=[B.dram_res(sc["kcT"])], dma=True)
        for s in range(4):
            S.op("dve", _copy(VCST[:, s, :], B.bank(1 + s)[:, 0:128]), reads=[B.bank_res[1 + s]], writes=[r_VCST])
        S.op("sp", _dma(sc["vc"][tsl, :].rearrange("(s p) d -> p s d", p=128), VCST), reads=[r_VCST],
             writes=[B.dram_res(sc["vc"])], dma=True)
    kmf = KM.rearrange("p a b -> p (a b)")
    S.op("dve", _ts(kmf, kmf, 1.0 / 256.0, None, ALU.mult), reads=[r_KM], writes=[r_KM])
    S.op("sp", _dma(sc["kmT"], kmf), reads=[r_KM], writes=[B.dram_res(sc["kmT"])], dma=True)
    B.top = base


def mixB_stage(B, C, l, bconst, fconst, sc, do_moba=True, do_swa=True, NH=8, NQT=4):
    S = B.S
    S.barrier()
    base = B.top
    BC = B.alloc(NBC * 2, BF16, (NBC,)); r_BC = Res("bconst")
    FC = B.alloc(NFC * 4, F32, (NFC,)); r_FC = Res("fconst")
    for c0 in range(0, NBC, 2048):
        c1 = min(NBC, c0 + 2048)
        S.op("sp", _dma(BC[:, c0:c1], bconst[:, c0:c1]), writes=[r_BC], dma=True)
    for c0 in range(0, NFC, 1024):
        c1 = min(NFC, c0 + 1024)
        S.op("sp", _dma(FC[:, c0:c1], fconst[:, c0:c1]), writes=[r_FC], dma=True)
    IDENT = BC[:, BC_IDENT:BC_IDENT + 128]
    PT = [B.alloc(1024, BF16, (512,)) for _ in range(3)]
    r_PT = [Res(f"PT{i}") for i in range(3)]
    YB = B.alloc(4096, BF16, (2048,)); r_YB = Res("YB")
    QT = B.alloc(4096, BF16, (2048,)); r_QT = Res("QT")

    if do_moba:
        KT = B.alloc(16384, BF16, (4, 2048)); r_KT = Res("KT")
        V = B.alloc(16384, BF16, (64, 128)); r_V = Res("V")
        KTo = B.alloc(4096, BF16, (2048,)); r_KTo = Res("KTo")
        Vo = B.alloc(4096, BF16, (16, 128)); r_Vo = Res("Vo")
        KM = B.alloc(64, BF16, (4, 8)); r_KMh = Res("KMh")
        KMF = B.alloc(128, F32, (4, 8)); r_KMF = Res("KMF")
        SELBT = B.alloc(4096, BF16, (2048,)); r_SELBT = Res("SELBT")
        S.op("dve", lambda e: e.memset(SELBT, 0.0), writes=[r_SELBT])
        G = B.alloc(128, F32, (32,)); T8 = B.alloc(32, F32, (8,)); SELB = B.alloc(64, BF16, (32,))
        r_G = Res("G")
        for h in range(NH):
            hs = slice(h * 128, (h + 1) * 128)
            S.op("sp", _dma(KT, sc["kbT_all"].rearrange("(r f) t -> f r t", r=4)[hs, :, :]),
                 reads=[B.dram_res(sc["kbT_all"])], writes=[r_KT], dma=True)
            for r4 in range(4):
                S.op("sp", _dma(V[:, r4 * 16:(r4 + 1) * 16, :],
                                sc["vb_all"][r4 * 2048:(r4 + 1) * 2048, hs].rearrange("(c p) d -> p c d", p=128)),
                     reads=[B.dram_res(sc["vb_all"])], writes=[r_V], dma=True)
            S.op("sp", _dma(KTo, sc["kbT"][hs, :]), reads=[B.dram_res(sc["kbT"])], writes=[r_KTo], dma=True)
            S.op("sp", _dma(Vo, sc["vb"][:, hs].rearrange("(c p) d -> p c d", p=128)),
                 reads=[B.dram_res(sc["vb"])], writes=[r_Vo], dma=True)
            S.op("sp", _dma(QT, sc["qbT"][hs, :]), reads=[B.dram_res(sc["qbT"])], writes=[r_QT], dma=True)
            S.op("sp", _dma(KMF, sc["kmT_all"].rearrange("(r p) c -> p r c", p=128)[:, :, h * 8:(h + 1) * 8]),
                 reads=[B.dram_res(sc["kmT_all"])], writes=[r_KMF], dma=True)
            S.op("dve", _copy(KM, KMF), reads=[r_KMF], writes=[r_KMh])
            for qs in range(4 * NQT):
                gb, gres = B.bank(7), B.bank_res[7]
                S.op("pe", _mm(gb[:, 0:32], QT[:, qs * 128:(qs + 1) * 128], KM.rearrange("p a b -> p (a b)"), True, True),
                     reads=[r_QT, r_KMh], writes=[gres])
                S.op("dve", _tt(G, gb[:, 0:32], FC[:, FC_PASTB + qs * 32:FC_PASTB + (qs + 1) * 32], ALU.add),
                     reads=[gres, r_FC], writes=[r_G])
                S.op("dve", lambda e: e.max(T8, G), reads=[r_G], writes=[r_G])
                S.op("dve", _ts(T8[:, 2:3], T8[:, 2:3], -1e29, None, ALU.max), reads=[r_G], writes=[r_G])
                S.op("dve", _ts(SELB, G, T8[:, 2:3], NEG, ALU.is_lt, ALU.mult), reads=[r_G], writes=[r_G])
                tb = B.bank(6, BF16)
                S.op("pe", (lambda e, tb=tb: e.transpose(tb[0:32, 0:128], SELB, IDENT)), reads=[r_G, r_BC],
                     writes=[B.bank_res[6]])
                S.op("act", _act(SELBT[0:32, qs * 128:(qs + 1) * 128], tb[0:32, 0:128], AF.Copy),
                     reads=[B.bank_res[6]], writes=[r_SELBT])
                if "dbg_g" in sc and h == 0:
                    S.op("sp", _dma(sc["dbg_g"][:, qs * 32:(qs + 1) * 32], G), reads=[r_G], writes=[B.dram_res(sc["dbg_g"])], dma=True)
                    S.op("sp", _dma(sc["dbg_t8"][:, qs * 8:(qs + 1) * 8], T8), reads=[r_G], writes=[B.dram_res(sc["dbg_t8"])], dma=True)
                    S.op("sp", _dma(sc["dbg_selb"][:, qs * 32:(qs + 1) * 32], SELB), reads=[r_G], writes=[B.dram_res(sc["dbg_selb"])], dma=True)
            if "dbg_g" in sc and h == 0:
                S.op("sp", _dma(sc["dbg_selbt"], SELBT), reads=[r_SELBT], writes=[B.dram_res(sc["dbg_selbt"])], dma=True)
            for qt in range(NQT):
                qsl = slice(qt * 512, (qt + 1) * 512)
                chunks = []
                for kc in range(64):
                    chunks.append(("f", KT[:, kc // 16, (kc % 16) * 128:(kc % 16 + 1) * 128], V[:, kc, :], kc // 2,
                                   [r_KT], [r_V]))
                for d4 in range(4):
                    ci = qt * 4 + d4
                    chunks.append(("d", KTo[:, ci * 128:(ci + 1) * 128], Vo[:, ci, :], d4, [r_KTo], [r_Vo]))
                nchk = len(chunks)
                bo, bd = 3, 4

                def qk(i):
                    kind, kap, vap, idx, rk, rv = chunks[i]
                    bs = i % 3
                    S.op("pe", _mm(B.bank(bs), kap, QT[:, qsl], True, False), reads=rk + [r_QT], writes=[B.bank_res[bs]])
                    if kind == "f":
                        S.op("pe", _mm(B.bank(bs), BC[:, BC_EALL + idx * 128:BC_EALL + (idx + 1) * 128],
                                       SELBT[:, qsl], False, True),
                             reads=[r_BC, r_SELBT], writes=[B.bank_res[bs]])
                    else:
                        S.op("pe", _mm(B.bank(bs), IDENT, BC[:, BC_CB + idx * 512:BC_CB + (idx + 1) * 512], False, True),
                             reads=[r_BC], writes=[B.bank_res[bs]])
                    col = FC_ALB + (h * 4 + qt) * 68 + i
                    S.op("act", _act(PT[bs], B.bank(bs), AF.Exp, bias=FC[:, col:col + 1]),
                         reads=[B.bank_res[bs], r_FC], writes=[r_PT[bs]])

                def pv(i):
                    kind, kap, vap, idx, rk, rv = chunks[i]
                    bs = i % 3
                    S.op("pe", _mm(B.bank(bo), vap, PT[bs], i == 0, i == nchk - 1), reads=rv + [r_PT[bs]],
                         writes=[B.bank_res[bo]])
                    S.op("pe", _mm(B.bank(bd), C.ones, PT[bs], i == 0, i == nchk - 1), reads=[r_PT[bs], C.r_const],
                         writes=[B.bank_res[bd]])
                qk(0)
                for i in range(nchk):
                    if i + 1 < nchk:
                        qk(i + 1)
                    pv(i)
                tm, rtm = next_tmp(C)
                S.op("dve", lambda e, tm=tm, bd=bd: e.reciprocal(tm, B.bank(bd)), reads=[B.bank_res[bd]], writes=[rtm])
                S.op("dve", _tt(YB[:, qsl], B.bank(bo), tm, ALU.mult), reads=[B.bank_res[bo], rtm], writes=[r_YB])
            S.op("sp", _dma(sc["ybT"][hs, :], YB), reads=[r_YB], writes=[B.dram_res(sc["ybT"])], dma=True)

    if do_swa:
        KCT = B.alloc(4352, BF16, (2176,)); r_KCT = Res("KCT")
        VCt = B.alloc(4352, BF16, (17, 128)); r_VCt = Res("VCt")
        VPA = B.alloc(4352, BF16, (17, 128)); VPB = B.alloc(4352, BF16, (17, 128)); r_VP = Res("VP")
        HK = B.alloc(1024, BF16, (4, 128)); HV = B.alloc(1024, BF16, (4, 128)); r_H = Res("halo")
        SINKE = B.alloc(32, F32, (8,)); r_SK = Res("sinke")
        TS = B.alloc(2048, F32, (512,)); r_TS = Res("TS")
        S.op("sp", _dma(KCT[:, 128:2176], sc["kcT"]), reads=[B.dram_res(sc["kcT"])], writes=[r_KCT], dma=True)
        S.op("sp", _dma(VCt[:, 1:17, :], sc["vc"].rearrange("(c p) d -> p c d", p=128)),
             reads=[B.dram_res(sc["vc"])], writes=[r_VCt], dma=True)
        S.op("sp", _dma(HK, sc["kcT_all"].rearrange("(r f) t -> f r t", r=4)[:, :, 1920:2048]),
             reads=[B.dram_res(sc["kcT_all"])], writes=[r_H], dma=True)
        S.op("sp", _dma(HV, sc["vc_all"].rearrange("(r c p) d -> p r c d", r=4, p=128)[:, :, 15, :]),
             reads=[B.dram_res(sc["vc_all"])], writes=[r_H], dma=True)
        for (dst, src, rd) in ((KCT[:, 0:128], HK, r_KCT), (VCt[:, 0, :], HV, r_VCt)):
            S.op("dve", _ts(dst, src[:, 0, :], FC[:, FC_HALOW:FC_HALOW + 1], None, ALU.mult), reads=[r_H, r_FC], writes=[rd])
            for r4 in range(1, 4):
                S.op("dve", _stt(dst, src[:, r4, :], FC[:, FC_HALOW + r4:FC_HALOW + r4 + 1], dst, ALU.mult, ALU.add),
                     reads=[r_H, r_FC, rd], writes=[rd])
        SWA_STOP = int(os.environ.get("SWA_STOP", "9"))
        KA = B.alloc(4352, BF16, (2176,)); KB = B.alloc(4352, BF16, (2176,)); r_KAB = Res("KAB")
        S.op("dve", lambda e: e.memset(KA, 0.0), writes=[r_KAB])
        S.op("dve", lambda e: e.memset(KB, 0.0), writes=[r_KAB])
        S.op("dve", _copy(KA[0:64, :], KCT[0:64, :]), reads=[r_KCT], writes=[r_KAB])
        S.op("dve", _copy(KB[64:128, :], KCT[64:128, :]), reads=[r_KCT], writes=[r_KAB])
        S.op("dve", lambda e: e.memset(VPA, 0.0), writes=[r_VP])
        S.op("dve", lambda e: e.memset(VPB, 0.0), writes=[r_VP])
        S.op("dve", _copy(VPA[:, :, 0:64], VCt[:, :, 0:64]), reads=[r_VCt], writes=[r_VP])
        S.op("dve", _copy(VPB[:, :, 64:128], VCt[:, :, 64:128]), reads=[r_VCt], writes=[r_VP])
        S.op("act", _act(SINKE, C.gains[:, SINK0 + l * 8:SINK0 + (l + 1) * 8], AF.Exp), reads=[C.r_gains], writes=[r_SK])
        ONEA = BC[:, BC_ONEA:BC_ONEA + 128]
        ONEB = BC[:, BC_ONEB:BC_ONEB + 128]
        for j in range(8 if SWA_STOP >= 2 else 0):
            S.op("sp", _dma(QT, sc["qcT"][j * 128:(j + 1) * 128, :]), reads=[B.dram_res(sc["qcT"])], writes=[r_QT], dma=True)
            for b in range(4 * NQT if SWA_STOP >= 3 else 0):
                bs = b % 2
                sb, sres = B.bank(bs), B.bank_res[bs]
                bo, bd = 2 + (b // 4) % 2, 4 + (b // 4) % 2
                for hf in range(2):
                    psl = slice(hf * 64, (hf + 1) * 64)
                    for w in range(2):
                        S.op("pe", _mm(sb[:, (hf * 2 + w) * 128:(hf * 2 + w + 1) * 128],
                                       (KA if hf == 0 else KB)[:, (b + w) * 128:(b + w + 1) * 128],
                                       QT[:, b * 128:(b + 1) * 128], True, True),
                             reads=[r_KAB, r_QT], writes=[sres])
                var = 1 if b == 0 else 0
                col = FC_SWAB + (j * 2 + var) * 512
                S.op("dve", _tt(TS, sb, FC[:, col:col + 512], ALU.add), reads=[sres, r_FC], writes=[r_TS])
                pt, rpt = PT[b % 3], r_PT[b % 3]
                S.op("act", _act(pt, TS, AF.Exp), reads=[r_TS], writes=[rpt])
                if SWA_STOP < 4:
                    continue
                osl = slice((b % 4) * 128, (b % 4 + 1) * 128)
                mats = [(VPA[:, b, :], ONEA, 0), (VPA[:, b + 1, :], ONEA, 1), (VPB[:, b, :], ONEB, 2), (VPB[:, b + 1, :], ONEB, 3)]
                for i, (vp, on, c4) in enumerate(mats):
                    S.op("pe", _mm(B.bank(bo)[:, osl], vp, pt[:, c4 * 128:(c4 + 1) * 128], i == 0, i == 3),
                         reads=[r_VP, rpt], writes=[B.bank_res[bo]])
                for i, (vp, on, c4) in enumerate(mats):
                    S.op("pe", _mm(B.bank(bd)[:, osl], on, pt[:, c4 * 128:(c4 + 1) * 128], i == 0, i == 3),
                         reads=[r_BC, rpt], writes=[B.bank_res[bd]])
                if b % 4 == 3:
                    tm, rtm = next_tmp(C)
                    S.op("dve", _ts(tm, B.bank(bd), C_sink(SINKE, j), None, ALU.add), reads=[B.bank_res[bd], r_SK], writes=[rtm])
                    S.op("dve", lambda e, tm=tm: e.reciprocal(tm, tm), reads=[rtm], writes=[rtm])
                    S.op("dve", _tt(YB[:, (b // 4) * 512:(b // 4 + 1) * 512], B.bank(bo), tm, ALU.mult),
                         reads=[B.bank_res[bo], rtm], writes=[r_YB])
            S.op("sp", _dma(sc["ycT"][j * 128:(j + 1) * 128, :], YB), reads=[r_YB], writes=[B.dram_res(sc["ycT"])], dma=True)
    B.top = base


def C_sink(SINKE, j):
    return SINKE[:, j:j + 1]


def mixC_stage(B, C, l, x_in, x_out, yscr, w_gate, w_branch, w_out, sc):
    S = B.S
    S.barrier()
    base = B.top
    X = B.alloc(65536, F32, (32, 512))
    r_X = [Res(f"X{q}") for q in range(4)]
    HN = B.alloc(32768, BF16, (32, 512))
    r_HN = [Res(f"HN{c}") for c in range(NCH)]
    YT = [B.alloc(8192, BF16, (8, 512)) for _ in range(3)]
    r_YT = [Res(f"YT{n}") for n in range(3)]
    SGT = B.alloc(8192, F32, (4, 512)); r_SGT = [Res(f"SGT{j}") for j in range(4)]
    ACC = B.alloc(8192, F32, (4, 512)); r_ACC = [Res(f"ACC{j}") for j in range(4)]
    MERGED = X.rearrange("p a b -> p (a b)")[:, 0:8192].bitcast(BF16).rearrange("p (a b) -> p a b", a=32)
    r_M = [Res(f"M{c}") for c in range(NCH)]
    for c in range(NCH):
        for q in range(4):
            alias(r_M[c], r_X[q])
    ysrc = [sc["yaT"], sc["ybT"], sc["ycT"]]
    for t in range(B.NT):
        tsl = slice(t * TT, (t + 1) * TT)
        load_x_tile(B, C, X, r_X, x_in, t)
        make_hn(B, C, X, r_X, HN, r_HN, gcol(l, 2))
        for n in range(3):
            S.op("sp", _dma(YT[n], ysrc[n][:, tsl].rearrange("(k p) t -> p k t", p=128)),
                 reads=[B.dram_res(ysrc[n])], writes=[r_YT[n]], dma=True)
        for mg in range(8):
            for n in range(3):
                def consume_g(k, view, sres_slot):
                    for j in range(4):
                        S.op("pe", _mm(B.bank(j), view(j * 128, 128), HN[:, k, :], k == 0, k == NCH - 1),
                             reads=[sres_slot, r_HN[k]], writes=[B.bank_res[j]])
                stream_weights(B, C, w_gate, 0, NCH, [(n * D + mg * 512, 512)], consume_g)

                def consume_b(k, view, sres_slot, n=n):
                    for j in range(4):
                        S.op("pe", _mm(B.bank(4 + j), view(j * 128, 128), YT[n][:, k, :], k == 0, k == 7),
                             reads=[sres_slot, r_YT[n]], writes=[B.bank_res[4 + j]])
                stream_weights(B, C, w_branch, n * BW, 8, [(mg * 512, 512)], consume_b)
                for j in range(4):
                    m = mg * 4 + j
                    bcol = BG0 + l * 96 + n * 32 + m
                    S.op("act", _act(SGT[:, j, :], B.bank(j), AF.Sigmoid, bias=C.gains[:, bcol:bcol + 1]),
                         reads=[B.bank_res[j], C.r_gains], writes=[r_SGT[j]])
                    if n == 0:
                        S.op("dve", _tt(ACC[:, j, :], SGT[:, j, :], B.bank(4 + j), ALU.mult),
                             reads=[r_SGT[j], B.bank_res[4 + j]], writes=[r_ACC[j]])
                    else:
                        S.op("dve", _tt(SGT[:, j, :], SGT[:, j, :], B.bank(4 + j), ALU.mult),
                             reads=[r_SGT[j], B.bank_res[4 + j]], writes=[r_SGT[j]])
                        if n == 1:
                            S.op("dve", _tt(ACC[:, j, :], ACC[:, j, :], SGT[:, j, :], ALU.add),
                                 reads=[r_SGT[j], r_ACC[j]], writes=[r_ACC[j]])
                        else:
                            S.op("dve", _tt(MERGED[:, m, :], ACC[:, j, :], SGT[:, j, :], ALU.add),
                                 reads=[r_SGT[j], r_ACC[j]], writes=[r_M[m]])
        out_groups(B, C, w_out, 0, [MERGED[:, k, :] for k in range(NCH)], r_M, yscr)
        residual_tail(B, C, t, x_in, x_out, yscr, gcol(l, 3), False)
    B.top = base


def _slopes():
    i = np.arange(1, 25, dtype=np.float32)
    s = np.exp2(-8.0 * i / 24.0).astype(np.float32)
    return s[:16], s[16:]


_QC_PERM = np.concatenate([np.concatenate([j * 64 + np.arange(64), (8 + j) * 64 + np.arange(64)]) for j in range(8)])


def make_bconst():
    bc = np.zeros((128, NBC), np.float32)
    bc[:, BC_IDENT:BC_IDENT + 128] = np.eye(128)
    p = np.arange(128)
    bc[:, BC_TRI:BC_TRI + 128] = (p[:, None] <= p[None, :])
    for n in range(32):
        bc[n, BC_EALL + n * 128:BC_EALL + (n + 1) * 128] = 1.0
    q = np.arange(512)
    for d4 in range(4):
        ok = ((d4 // 2) == (q[None, :] // 256)) & ((d4 * 128 + p[:, None]) <= q[None, :])
        bc[:, BC_CB + d4 * 512:BC_CB + (d4 + 1) * 512] = np.where(ok, 0.0, NEG)
    bc[:, BC_ONEA:BC_ONEA + 64] = 1.0
    bc[:, BC_ONEB + 64:BC_ONEB + 128] = 1.0
    return bc.astype(ml_dtypes.bfloat16)


def make_fconst(r):
    fc = np.zeros((128, NFC), np.float32)
    swa_s, moba_s = _slopes()
    p = np.arange(128, dtype=np.float32)
    for h in range(8):
        for qt in range(4):
            q0 = r * 2048 + qt * 512
            c0 = FC_ALB + (h * 4 + qt) * 68
            for kc in range(64):
                fc[:, c0 + kc] = moba_s[h] * (kc * 128 + p - q0)
            for d4 in range(4):
                fc[:, c0 + 64 + d4] = moba_s[h] * (d4 * 128 + p)
    for qs in range(16):
        blk = r * 8 + qs // 2
        n = np.arange(32)
        fc[:, FC_PASTB + qs * 32:FC_PASTB + (qs + 1) * 32] = np.where(n < blk, 0.0, -1e30)[None, :]
    s_loc = np.arange(128)[:, None]
    t_loc = np.arange(128)[None, :]
    d_cur = (t_loc - s_loc).astype(np.float32)
    d_prev = (t_loc + 128 - s_loc).astype(np.float32)
    for j in range(8):
        for var in range(2):
            c0 = FC_SWAB + (j * 2 + var) * 512
            for hf, h in enumerate((j, 8 + j)):
                prev = np.where(d_prev < 128, -swa_s[h] * d_prev, -1e30)
                if var == 1 and r == 0:
                    prev = np.full((128, 128), -1e30, np.float32)
                cur = np.where(d_cur >= 0, -swa_s[h] * d_cur, -1e30)
                fc[:, c0 + (hf * 2) * 128:c0 + (hf * 2 + 1) * 128] = prev
                fc[:, c0 + (hf * 2 + 1) * 128:c0 + (hf * 2 + 2) * 128] = cur
    if r > 0:
        fc[:, FC_HALOW + r - 1] = 1.0
    return fc


def pvec(v):
    return np.ascontiguousarray(np.asarray(v, np.float32).reshape(-1, 128).T)


def make_gains(inp):
    g = np.zeros((128, NG), np.float32)
    names = ["ffn1_pre_g", "ffn1_post_g", "mix_pre_g", "mix_post_g", "ffn2_pre_g", "ffn2_post_g"]
    for l in range(2):
        for i, nm in enumerate(names):
            g[:, gcol(l, i):gcol(l, i) + 32] = pvec(inp[nm][l])
        g[:, BG0 + l * 96:BG0 + (l + 1) * 96] = pvec(inp["b_gate"][l])
        sk = np.asarray(inp["swa_sinks"][l], np.float32)
        g[:64, SINK0 + l * 8:SINK0 + (l + 1) * 8] = sk[None, 0:8]
        g[64:, SINK0 + l * 8:SINK0 + (l + 1) * 8] = sk[None, 8:16]
    return g


def layer_arrays(inp, l, skip_ffn=False):
    w_in = np.array(inp["w_in"][l], np.float32, copy=True)
    w_in[:, 5120:6144] = w_in[:, 5120 + _QC_PERM]
    wb = np.array(inp["w_branch"][l], np.float32, copy=True)
    wb[2] = wb[2][_QC_PERM, :]
    a = {} if skip_ffn else {
        f"ffn1_w_up_{l}": inp["ffn1_w_up"][l], f"ffn1_w_down_{l}": inp["ffn1_w_down"][l],
        f"ffn2_w_up_{l}": inp["ffn2_w_up"][l], f"ffn2_w_down_{l}": inp["ffn2_w_down"][l]}
    a.update({
        f"w_in_{l}": w_in, f"w_gate_{l}": inp["w_gate"][l], f"w_branch_{l}": wb.reshape(3 * BW, D),
        f"w_out_{l}": inp["w_out"][l],
        f"wsT_{l}": np.ascontiguousarray(np.asarray(inp["gmlp_w_s"][l], np.float32).transpose(2, 0, 1)),
        f"rowtab_{l}": np.ascontiguousarray(np.broadcast_to(np.stack([
            np.asarray(inp["gmlp_b_s"][l], np.float32).reshape(1024),
            np.asarray(inp["gmlp_ln_g"][l], np.float32), np.asarray(inp["gmlp_ln_b"][l], np.float32)])[None], (128, 3, 1024))),
    })
    return {k: np.ascontiguousarray(np.asarray(v, np.float32)) for k, v in a.items()}


SCRATCH = {
    "qbT": ([BW, TOK], BF16), "kbT": ([BW, TOK], BF16), "vb": ([TOK, BW], BF16), "kmT": ([128, 64], F32),
    "qcT": ([BW, TOK], BF16), "kcT": ([128, TOK], BF16), "vc": ([TOK, 128], BF16), "yaT": ([BW, TOK], BF16),
    "ybT": ([BW, TOK], BF16), "ycT": ([BW, TOK], BF16),
    "kbT_all": ([4 * BW, TOK], BF16), "vb_all": ([4 * TOK, BW], BF16), "kmT_all": ([512, 64], F32),
    "kcT_all": ([4 * 128, TOK], BF16), "vc_all": ([4 * TOK, 128], BF16),
}
WSHAPES = {"ffn1_w_up": [D, 2 * DFF], "ffn1_w_down": [DFF, D], "ffn2_w_up": [D, 2 * DFF], "ffn2_w_down": [DFF, D],
           "w_in": [D, IN_COLS], "w_gate": [D, 3 * D], "w_branch": [3 * BW, D], "w_out": [D, D],
           "wsT": [128, 8, 128], "rowtab": [128, 3, 1024]}


def build_launch(stages, ext_in, ext_out, NT=4, mixb_kw=None):
    def stage_fn(nc, B):
        made = {}

        def T(name, shape, dtype):
            if name not in made:
                kind = "ExternalInput" if name in ext_in else ("ExternalOutput" if name in ext_out else "Internal")
                made[name] = nc.dram_tensor(name, shape, dtype, kind=kind).ap()
            return made[name]

        def W(nm, l):
            return T(f"{nm}_{l}", WSHAPES[nm], F32)

        def SC(l):
            d = {k: T(f"{k}_{l}", sh, dt) for k, (sh, dt) in SCRATCH.items()}
            if os.environ.get("MIXB_DBG"):
                d["dbg_g"] = T("dbg_g", [128, 512], F32); d["dbg_t8"] = T("dbg_t8", [128, 128], F32)
                d["dbg_selb"] = T("dbg_selb", [128, 512], BF16); d["dbg_selbt"] = T("dbg_selbt", [128, 2048], BF16)
            return d
        bconst = T("bconst", [128, NBC], BF16)
        fconst = T("fconst", [128, NFC], F32)
        gains = T("gains", [128, NG], F32)
        C = setup_common(B, bconst, gains)
        yscr = T("yscr", [D, TT], F32)
        outs = []
        for st in stages:
            if st[0] == "ffn":
                _, l, which, xi, xo = st
                pre, post = (0, 1) if which == 1 else (4, 5)
                ffn_stage(B, C, T(xi, [D, TOK], F32), T(xo, [D, TOK], F32), yscr,
                          W(f"ffn{which}_w_up", l), W(f"ffn{which}_w_down", l), gcol(l, pre), gcol(l, post))
            elif st[0] == "mixA":
                _, l, xi = st
                mixA_stage(B, C, l, T(xi, [D, TOK], F32), W("w_in", l), W("wsT", l), W("rowtab", l), bconst, SC(l))
            elif st[0] == "mixB":
                _, l = st
                mixB_stage(B, C, l, bconst, fconst, SC(l), **(mixb_kw or {}))
            elif st[0] == "mixC":
                _, l, xi, xo = st
                mixC_stage(B, C, l, T(xi, [D, TOK], F32), T(xo, [D, TOK], F32), yscr,
                           W("w_gate", l), W("w_branch", l), W("w_out", l), SC(l))
        final_wait(B, [B.dram_res(made[n]) for n in made if n in ext_out])
    return make_program(stage_fn, NT=NT)


OWN = ["qbT", "kbT", "vb", "kmT", "qcT", "kcT", "vc", "yaT"]
GATH = ["kbT", "vb", "kmT", "kcT", "vc"]


def run_launch(nc, in_maps, n_cores=8):
    res = run_bass_kernel_spmd(nc, in_maps, core_ids=list(range(n_cores)))
    return res.results


def gather_maps(outs, l, n_cores=8):
    g = []
    for c in range(n_cores):
        b0 = (c // 4) * 4
        d = {}
        for k in GATH:
            d[f"{k}_all_{l}"] = np.concatenate([outs[b0 + r][f"{k}_{l}"] for r in range(4)], axis=0)
        g.append(d)
    return g


def kernel(**inp):
    inp = {k: np.asarray(v) for k, v in inp.items()}
    n = 8
    bconst = make_bconst()
    gains = make_gains(inp)
    fcs = [make_fconst(c % 4) for c in range(n)]
    LA = [layer_arrays(inp, 0), layer_arrays(inp, 1)]
    x = inp["x"]
    base = [{"bconst": bconst, "gains": gains, "fconst": fcs[c]} for c in range(n)]
    consts = {"bconst", "gains", "fconst"}
    own0 = {f"{k}_0" for k in OWN}
    own1 = {f"{k}_1" for k in OWN}
    all0 = {f"{k}_all_0" for k in GATH}
    all1 = {f"{k}_all_1" for k in GATH}

    def wnames(l, names):
        return {f"{nm}_{l}" for nm in names}

    w1 = wnames(0, ["ffn1_w_up", "ffn1_w_down", "w_in", "wsT", "rowtab"])
    nc1 = build_launch([("ffn", 0, 1, "x0", "x1"), ("mixA", 0, "x1")], consts | {"x0"} | w1, {"x1"} | own0)
    maps = []
    for c in range(n):
        m = dict(base[c])
        m["x0"] = np.ascontiguousarray(x[c // 4, (c % 4) * TOK:(c % 4 + 1) * TOK, :].T)
        m.update({k: LA[0][k] for k in w1})
        maps.append(m)
    o1 = run_launch(nc1, maps)
    del maps
    w2 = wnames(0, ["w_gate", "w_branch", "w_out", "ffn2_w_up", "ffn2_w_down"]) | \
        wnames(1, ["ffn1_w_up", "ffn1_w_down", "w_in", "wsT", "rowtab"])
    nc2 = build_launch([("mixB", 0), ("mixC", 0, "x1", "x2"), ("ffn", 0, 2, "x2", "x3"), ("ffn", 1, 1, "x3", "x4"),
                        ("mixA", 1, "x4")], consts | {"x1"} | own0 | all0 | w2, {"x4"} | own1)
    g1 = gather_maps(o1, 0)
    maps = []
    for c in range(n):
        m = dict(base[c])
        m["x1"] = o1[c]["x1"]
        m.update({k: o1[c][k] for k in own0})
        m.update(g1[c])
        for k in w2:
            m[k] = LA[int(k[-1])][k]
        maps.append(m)
    o2 = run_launch(nc2, maps)
    del maps, o1, g1
    w3 = wnames(1, ["w_gate", "w_branch", "w_out", "ffn2_w_up", "ffn2_w_down"])
    nc3 = build_launch([("mixB", 1), ("mixC", 1, "x4", "x5"), ("ffn", 1, 2, "x5", "xout")],
                       consts | {"x4"} | own1 | all1 | w3, {"xout"})
    g2 = gather_maps(o2, 1)
    maps = []
    for c in range(n):
        m = dict(base[c])
        m["x4"] = o2[c]["x4"]
        m.update({k: o2[c][k] for k in own1})
        m.update(g2[c])
        m.update({k: LA[1][k] for k in w3})
        maps.append(m)
    o3 = run_launch(nc3, maps)
    out = np.empty_like(x)
    for c in range(n):
        out[c // 4, (c % 4) * TOK:(c % 4 + 1) * TOK, :] = o3[c]["xout"].T
    return out
```

```python
import contextlib
import os
import numpy as np
import ml_dtypes
import concourse.bass as bass
import concourse.mybir as mybir
from concourse.bass_utils import run_bass_kernel_spmd

F32 = mybir.dt.float32
BF16 = mybir.dt.bfloat16
AF = mybir.ActivationFunctionType
ALU = mybir.AluOpType
AX = mybir.AxisListType

D = 4096
NCH = D // 128
TT = 512
TOK = 2048
DFF = 8192
BW = 1024
IN_COLS = 6400
RMS_EPS = 1e-6
LN_EPS = 1e-5
NEG = -30000.0
SEM_ROT = 28000
SAME_ENGINE_SYNC = True
NSLOT = 5
WCACHE = True


class Res:
    __slots__ = ("name", "last_write", "readers", "dma_readers", "aliases", "sem", "cnt")

    def __init__(self, name):
        self.name = name
        self.last_write = None
        self.readers = {}
        self.dma_readers = []
        self.aliases = []
        self.sem = None
        self.cnt = 0


def alias(a, b):
    a.aliases.append(b)
    b.aliases.append(a)


class Op:
    __slots__ = ("eng", "fn", "deps", "tick", "token", "is_dma", "dres")

    def __init__(self, eng, fn, is_dma, dres):
        self.eng = eng
        self.fn = fn
        self.deps = []
        self.tick = False
        self.token = None
        self.is_dma = is_dma
        self.dres = dres


class Sched:
    ENGS = ("pe", "act", "dve", "pool", "sp")

    def __init__(self, sem_pool):
        self.sem_pool = list(sem_pool)
        self.ops = []
        self.by_eng = {e: [] for e in self.ENGS}
        self.pending_barrier = {e: None for e in self.ENGS}
        self.dma_since_barrier = []
        self.last_op = {e: None for e in self.ENGS}

    def op(self, eng, fn, reads=(), writes=(), dma=False):
        o = Op(eng, fn, dma, writes[0] if dma else None)
        deps = []
        for r in reads:
            if r.last_write is not None:
                deps.append(r.last_write)
            for a in r.aliases:
                if a.last_write is not None:
                    deps.append(a.last_write)
        for w in writes:
            for x in [w] + w.aliases:
                if x.last_write is not None:
                    deps.append(x.last_write)
                deps.extend(x.readers.values())
                deps.extend(x.dma_readers)
        pb = self.pending_barrier[eng]
        if pb is not None:
            deps.extend(pb)
            self.pending_barrier[eng] = None
        seen = set()
        for d in deps:
            if d is o or id(d) in seen:
                continue
            seen.add(id(d))
            if (not d.is_dma) and (not dma) and d.eng == eng:
                if eng == "pe" or not SAME_ENGINE_SYNC:
                    continue
            d.tick = True
            o.deps.append(d)
        for r in reads:
            if dma:
                r.dma_readers.append(o)
            else:
                r.readers[eng] = o
        for w in writes:
            w.last_write = o
            w.readers = {}
            w.dma_readers = []
        self.ops.append(o)
        self.by_eng[eng].append(o)
        self.last_op[eng] = o
        if dma:
            self.dma_since_barrier.append(o)
        return o

    def barrier(self):
        deps = [o for o in self.last_op.values() if o is not None and not o.is_dma]
        deps += self.dma_since_barrier
        self.dma_since_barrier = []
        for e in self.ENGS:
            old = self.pending_barrier[e] or []
            self.pending_barrier[e] = old + deps

    def number(self):
        eng_sem = {}
        eng_cnt = {}
        for o in self.ops:
            if o.is_dma:
                r = o.dres
                if r.sem is None or r.cnt >= SEM_ROT:
                    r.sem = self.sem_pool.pop()
                    r.cnt = 0
                r.cnt += 16
                o.token = (r.sem, r.cnt)
            elif o.tick:
                e = o.eng
                if e not in eng_sem or eng_cnt[e] >= SEM_ROT:
                    eng_sem[e] = self.sem_pool.pop()
                    eng_cnt[e] = 0
                eng_cnt[e] += 1
                o.token = (eng_sem[e], eng_cnt[e])

    def emit(self, eng_name, eng):
        waited = {}
        for o in self.by_eng[eng_name]:
            for d in o.deps:
                sem, val = d.token
                k = id(sem)
                if waited.get(k, (None, 0))[1] < val:
                    eng.wait_ge(sem, val)
                    waited[k] = (sem, val)
            ins = o.fn(eng)
            if o.token is not None:
                ins.then_inc(o.token[0], 16 if o.is_dma else 1)


class Builder:
    def __init__(self, nc, S, arena, ps, NT):
        self.nc = nc
        self.S = S
        self.arena = arena
        self.ps = ps
        self.NT = NT
        self.top = 0
        self.bank_res = [Res(f"bank{i}") for i in range(8)]
        self.dram = {}
        self.caches = {}
        self.cache_res = None
        self.cur_t = 0
        self.uid = 0

    def alloc(self, nbytes, dtype, shape_free):
        assert nbytes % 4 == 0
        off = self.top // 4
        self.top += nbytes
        assert self.top <= self.arena_bytes, (self.top, self.arena_bytes)
        ap = self.arena[:, off:off + nbytes // 4]
        if dtype != F32:
            ap = ap.bitcast(dtype)
        if len(shape_free) == 2:
            ap = ap.rearrange("p (a b) -> p a b", a=shape_free[0])
        elif len(shape_free) == 3:
            ap = ap.rearrange("p (a b c) -> p a b c", a=shape_free[0], b=shape_free[1])
        return ap

    def bank(self, i, dtype=F32):
        ap = self.ps[:, i * 512:(i + 1) * 512]
        if dtype != F32:
            ap = ap.bitcast(dtype)
        return ap


def _mm(out, lhsT, rhs, start, stop):
    return lambda e: e.matmul(out, lhsT, rhs, start=start, stop=stop)


def _act(out, in_, func, bias=None, scale=None):
    kw = {}
    if bias is not None:
        kw["bias"] = bias
    if scale is not None:
        kw["scale"] = scale
    return lambda e: e.activation(out, in_, func, **kw)


def _dma(out, in_):
    return lambda e: e.dma_start(out=out, in_=in_)


def _ts(out, in0, s1, s2, op0, op1=None):
    if op1 is None:
        return lambda e: e.tensor_scalar(out, in0, s1, None, op0)
    return lambda e: e.tensor_scalar(out, in0, s1, s2, op0, op1)


def _tt(out, in0, in1, op):
    return lambda e: e.tensor_tensor(out, in0, in1, op)


def _stt(out, in0, scalar, in1, op0, op1):
    return lambda e: e.scalar_tensor_tensor(out, in0, scalar, in1, op0, op1)


def _copy(out, in_):
    return lambda e: e.tensor_copy(out, in_)


class Common:
    pass


def setup_common(B, consts_ap, gains_ap):
    S = B.S
    C = Common()
    C.ones = B.alloc(256, BF16, (128,))
    C.ident = B.alloc(256, BF16, (128,))
    C.r_const = Res("const")
    S.op("sp", _dma(C.ident, consts_ap[:, 0:128]), writes=[C.r_const], dma=True)
    S.op("dve", lambda e: e.memset(C.ones, 1.0), writes=[C.r_const])
    ng = gains_ap.shape[1]
    C.gains = B.alloc(ng * 4, F32, (ng,))
    C.r_gains = Res("gains")
    S.op("sp", _dma(C.gains, gains_ap), writes=[C.r_gains], dma=True)
    C.slots = [B.alloc(8192, BF16, (4, 1024)) for _ in range(NSLOT)]
    C.slot_res = [Res(f"slot{i}") for i in range(NSLOT)]
    C.slot_i = 0
    C.rstd = [B.alloc(2048, F32, (512,)) for _ in range(2)]
    C.r_rstd = [Res("rstd0"), Res("rstd1")]
    C.sq = [B.alloc(1024, BF16, (512,)) for _ in range(2)]
    C.r_sq = [Res("sq0"), Res("sq1")]
    C.tmp = [B.alloc(2048, F32, (512,)) for _ in range(4)]
    C.r_tmp = [Res(f"tmp{i}") for i in range(4)]
    C.tmp_i = 0
    return C


def next_tmp(C):
    i = C.tmp_i
    C.tmp_i = (i + 1) % 4
    return C.tmp[i], C.r_tmp[i]


def stream_weights(B, C, w_ap, row0, nk, col_ranges, consume):
    S = B.S
    nq = (nk + 3) // 4
    W = sum(n for _, n in col_ranges)
    use_cache = WCACHE and B.NT > 1 and getattr(B, "cache_res", None) is not None
    cache = None
    if use_cache:
        key = (w_ap.tensor.name, row0, tuple(col_ranges))
        if key not in B.caches:
            B.caches[key] = B.nc.dram_tensor(f"wc{len(B.caches)}_{B.uid}", [nq * 128, 4 * W], BF16, kind="Internal").ap()
        cache = B.caches[key]
    for q in range(nq):
        si = C.slot_i
        C.slot_i = (si + 1) % NSLOT
        slot = C.slots[si].rearrange("p a b -> p (a b)")
        sres = C.slot_res[si]
        kc_n = min(4, nk - q * 4)
        flat = slot[:, 0:kc_n * W]
        s3 = flat.rearrange("p (kc c) -> p kc c", kc=kc_n)
        if use_cache and B.cur_t > 0:
            S.op("pool", _dma(flat, cache[q * 128:(q + 1) * 128, 0:kc_n * W]), reads=[B.cache_res],
                 writes=[sres], dma=True)
        else:
            off = 0
            for (c0, n) in col_ranges:
                src = w_ap[row0 + q * 512: row0 + q * 512 + kc_n * 128, c0:c0 + n].rearrange(
                    "(kc p) c -> p kc c", p=128)
                S.op("pool", _dma(s3[:, :, off:off + n], src), writes=[sres], dma=True)
                off += n
            if use_cache:
                S.op("sp", _dma(cache[q * 128:(q + 1) * 128, 0:kc_n * W], flat), reads=[sres], writes=[B.cache_res],
                     dma=True)
        for kc in range(kc_n):
            k = q * 4 + kc
            consume(k, (lambda o, n, _s=slot, _kc=kc: _s[:, _kc * W + o:_kc * W + o + n]), sres)


def rms_stats(B, C, chunks, chunk_res, which, eps, bank_i):
    S = B.S
    bank = B.bank(bank_i)
    bres = B.bank_res[bank_i]
    n = len(chunks)
    for c in range(n):
        sq, rsq = C.sq[c % 2], C.r_sq[c % 2]
        S.op("act", _act(sq, chunks[c], AF.Square), reads=[chunk_res[c]], writes=[rsq])
        S.op("pe", _mm(bank, C.ones, sq, c == 0, c == n - 1), reads=[rsq, C.r_const], writes=[bres])
    rstd, rr = C.rstd[which], C.r_rstd[which]
    S.op("dve", _ts(rstd, bank, 1.0 / D, eps, ALU.mult, ALU.add), reads=[bres], writes=[rr])
    S.op("act", _act(rstd, rstd, AF.Sqrt), reads=[rr], writes=[rr])
    S.op("dve", lambda e: e.reciprocal(rstd, rstd), reads=[rr], writes=[rr])
    return rstd, rr


def load_x_tile(B, C, X, r_X, x_in, t):
    S = B.S
    for q in range(4):
        src = x_in[q * 1024:(q + 1) * 1024, t * TT:(t + 1) * TT].rearrange("(c p) t -> p c t", p=128)
        S.op("sp", _dma(X[:, q * 8:(q + 1) * 8, :], src), reads=[B.dram_res(x_in)], writes=[r_X[q]], dma=True)


def make_hn(B, C, X, r_X, HN, r_HN, gcol):
    S = B.S
    rstd, rr = rms_stats(B, C, [X[:, c, :] for c in range(NCH)], [r_X[c // 8] for c in range(NCH)], 0, RMS_EPS, 0)
    for c in range(NCH):
        S.op("dve", _stt(HN[:, c, :], X[:, c, :], C.gains[:, gcol + c:gcol + c + 1], rstd, ALU.mult, ALU.mult),
             reads=[r_X[c // 8], rr, C.r_gains], writes=[r_HN[c]])


def residual_tail(B, C, t, x_in, x_out, yscr, gcol, half):
    S = B.S
    rstd, rr = C.rstd[1], C.r_rstd[1]
    for c in range(NCH):
        ty, rty = next_tmp(C)
        tx, rtx = next_tmp(C)
        S.op("sp", _dma(ty, yscr[c * 128:(c + 1) * 128, :]), reads=[B.dram_res(yscr)], writes=[rty], dma=True)
        S.op("sp", _dma(tx, x_in[c * 128:(c + 1) * 128, t * TT:(t + 1) * TT]), reads=[B.dram_res(x_in)],
             writes=[rtx], dma=True)
        S.op("dve", _stt(ty, ty, C.gains[:, gcol + c:gcol + c + 1], rstd, ALU.mult, ALU.mult),
             reads=[rty, rr, C.r_gains], writes=[rty])
        if half:
            S.op("dve", _stt(tx, ty, 0.5, tx, ALU.mult, ALU.add), reads=[rty, rtx], writes=[rtx])
        else:
            S.op("dve", _tt(tx, ty, tx, ALU.add), reads=[rty, rtx], writes=[rtx])
        S.op("sp", _dma(x_out[c * 128:(c + 1) * 128, t * TT:(t + 1) * TT], tx), reads=[rtx],
             writes=[B.dram_res(x_out)], dma=True)


def out_groups(B, C, w_ap, row0, rhs_chunks, rhs_res, yscr, evac_extra=None):
    S = B.S
    nk = len(rhs_chunks)
    sbank, sres = B.bank(7), B.bank_res[7]
    groups = [list(range(i, min(i + 7, NCH))) for i in range(0, NCH, 7)]
    done = 0
    for grp in groups:
        c0 = grp[0] * 128
        ncols = len(grp) * 128

        def consume(k, view, sres_slot, grp=grp):
            for j, m in enumerate(grp):
                S.op("pe", _mm(B.bank(j), view(j * 128, 128), rhs_chunks[k], k == 0, k == nk - 1),
                     reads=[sres_slot, rhs_res[k]], writes=[B.bank_res[j]])
        stream_weights(B, C, w_ap, row0, nk, [(c0, ncols)], consume)
        for j, m in enumerate(grp):
            ty, rty = next_tmp(C)
            eng = "act" if (j % 2 == 0) else "dve"
            if eng == "act":
                S.op("act", _act(ty, B.bank(j), AF.Copy), reads=[B.bank_res[j]], writes=[rty])
            else:
                S.op("dve", _copy(ty, B.bank(j)), reads=[B.bank_res[j]], writes=[rty])
            sq, rsq = C.sq[done % 2], C.r_sq[done % 2]
            S.op("act", _act(sq, ty, AF.Square), reads=[rty], writes=[rsq])
            S.op("pe", _mm(sbank, C.ones, sq, done == 0, done == NCH - 1), reads=[rsq, C.r_const], writes=[sres])
            S.op("sp", _dma(yscr[m * 128:(m + 1) * 128, :], ty), reads=[rty], writes=[B.dram_res(yscr)], dma=True)
            done += 1
    rstd, rr = C.rstd[1], C.r_rstd[1]
    S.op("dve", _ts(rstd, sbank, 1.0 / D, RMS_EPS, ALU.mult, ALU.add), reads=[sres], writes=[rr])
    S.op("act", _act(rstd, rstd, AF.Sqrt), reads=[rr], writes=[rr])
    S.op("dve", lambda e: e.reciprocal(rstd, rstd), reads=[rr], writes=[rr])


def ffn_stage(B, C, x_in, x_out, yscr, w_up, w_down, g_pre_col, g_post_col):
    S = B.S
    S.barrier()
    base = B.top
    X = B.alloc(65536, F32, (32, 512))
    r_X = [Res(f"X{q}") for q in range(4)]
    HN = B.alloc(32768, BF16, (32, 512))
    r_HN = [Res(f"HN{c}") for c in range(NCH)]
    ACTB = X.rearrange("p a b -> p (a b)").bitcast(BF16).rearrange("p (a b) -> p a b", a=64)
    r_A = [Res(f"A{j}") for j in range(64)]
    for j in range(64):
        alias(r_A[j], r_X[j // 16])
    B.cache_res = Res("wcache"); B.caches = {}; B.uid += 1
    for t in range(B.NT):
        B.cur_t = t
        load_x_tile(B, C, X, r_X, x_in, t)
        make_hn(B, C, X, r_X, HN, r_HN, g_pre_col)
        for g in range(16):
            def consume(k, view, sres_slot):
                for j in range(8):
                    S.op("pe", _mm(B.bank(j), view(j * 128, 128), HN[:, k, :], k == 0, k == NCH - 1),
                         reads=[sres_slot, r_HN[k]], writes=[B.bank_res[j]])
            stream_weights(B, C, w_up, 0, NCH, [(g * 512, 512), (DFF + g * 512, 512)], consume)
            for j in range(4):
                ty, rty = next_tmp(C)
                S.op("act", _act(ty, B.bank(j), AF.Silu), reads=[B.bank_res[j]], writes=[rty])
                S.op("dve", _tt(ACTB[:, g * 4 + j, :], ty, B.bank(4 + j), ALU.mult),
                     reads=[rty, B.bank_res[4 + j]], writes=[r_A[g * 4 + j]])
        out_groups(B, C, w_down, 0, [ACTB[:, k, :] for k in range(64)], r_A, yscr)
        residual_tail(B, C, t, x_in, x_out, yscr, g_post_col, True)
    B.top = base


def _builder_extras():
    def dram_res(self, ap):
        name = ap.tensor.name if hasattr(ap, "tensor") else ap.name
        if name not in self.dram:
            self.dram[name] = Res("dram_" + name)
        return self.dram[name]
    Builder.dram_res = dram_res


_builder_extras()

ARENA_BYTES = 204 * 1024


def make_program(stage_fn, NT=4):
    nc = bass.Bass("TRN2", target_bir_lowering=False)
    with contextlib.ExitStack() as es:
        arena = es.enter_context(nc.sbuf_tensor("arena", [128, ARENA_BYTES // 4], F32))
        ps = es.enter_context(nc.psum_tensor("ps", [128, 4096], F32))
        sems = [es.enter_context(nc.semaphore(f"s{i}")) for i in range(90)]
        S = Sched(sems)
        B = Builder(nc, S, arena, ps, NT)
        B.arena_bytes = ARENA_BYTES
        stage_fn(nc, B)
        S.number()
        block = es.enter_context(nc.Block())

        @block.tensor
        def _(e):
            S.emit("pe", e)

        @block.scalar
        def _(e):
            S.emit("act", e)

        @block.vector
        def _(e):
            S.emit("dve", e)

        @block.gpsimd
        def _(e):
            S.emit("pool", e)

        @block.sync
        def _(e):
            S.emit("sp", e)
    return nc


def final_wait(B, out_res_list):
    B.S.op("sp", lambda e: None, reads=out_res_list)


def gcol(l, i):
    return (l * 6 + i) * 32


BG0 = 2 * 6 * 32
SINK0 = BG0 + 2 * 96
NG = SINK0 + 16

BC_IDENT = 0
BC_TRI = 128
BC_EALL = 256
BC_CB = BC_EALL + 4096
BC_ONEA = BC_CB + 2048
BC_ONEB = BC_ONEA + 128
NBC = BC_ONEB + 128
FC_ALB = 0
FC_PASTB = FC_ALB + 8 * 4 * 68
FC_SWAB = FC_PASTB + 512
FC_HALOW = FC_SWAB + 8 * 2 * 512
NFC = FC_HALOW + 4


def fm_group(B, C, w_ap, col0, nchunks, HN, r_HN, evac):
    S = B.S

    def consume(k, view, sres_slot):
        for j in range(nchunks):
            S.op("pe", _mm(B.bank(j), view(j * 128, 128), HN[:, k, :], k == 0, k == NCH - 1),
                 reads=[sres_slot, r_HN[k]], writes=[B.bank_res[j]])
    nc_ = nchunks * 128
    rngs = [(col0, nc_)] if nc_ <= 512 else [(col0, 512), (col0 + 512, nc_ - 512)]
    stream_weights(B, C, w_ap, 0, NCH, rngs, consume)
    for j in range(nchunks):
        evac(j)


def tm_group(B, C, w_ap, col0, HN, r_HN):
    S = B.S

    def consume(k, view, sres_slot):
        for s in range(4):
            for cb in range(2):
                S.op("pe", _mm(B.bank(s * 2 + cb), HN[:, k, s * 128:(s + 1) * 128], view(cb * 512, 512),
                               k == 0, k == NCH - 1),
                     reads=[sres_slot, r_HN[k]], writes=[B.bank_res[s * 2 + cb]])
    stream_weights(B, C, w_ap, 0, NCH, [(col0, 512), (col0 + 512, 512)], consume)


def mixA_stage(B, C, l, x_in, w_in, wsT, rowtab, bconst, sc):
    S = B.S
    S.barrier()
    base = B.top
    X = B.alloc(65536, F32, (32, 512))
    r_X = [Res(f"X{q}") for q in range(4)]
    HN = B.alloc(32768, BF16, (32, 512))
    r_HN = [Res(f"HN{c}") for c in range(NCH)]
    ROW = B.alloc(12288, F32, (3, 1024))
    r_ROW = Res("rowtab")
    WSF = B.alloc(4096, F32, (8, 128))
    WST = B.alloc(2048, BF16, (8, 128))
    TRI = B.alloc(256, BF16, (128,))
    r_WS = Res("wst")
    LNS = B.alloc(64, F32, (16,))
    r_LNS = Res("lns")
    KM = B.alloc(256, F32, (8, 8))
    r_KM = Res("km")
    xb = X.rearrange("p a b -> p (a b)")

    def sub(off, nbytes, dtype, shape):
        ap = xb[:, off // 4:(off + nbytes) // 4]
        if dtype != F32:
            ap = ap.bitcast(dtype)
        if len(shape) == 2:
            ap = ap.rearrange("p (a b) -> p a b", a=shape[0])
        return ap

    def xres(name):
        r = Res(name)
        for q in range(4):
            alias(r, r_X[q])
        return r
    U = sub(0, 8192, BF16, (8, 512)); r_U = xres("U")
    STG = [sub(8192, 8192, BF16, (8, 512)), sub(16384, 8192, BF16, (8, 512))]
    r_STG = [xres("STG0"), xres("STG1")]
    VG = [sub(24576, 4096, F32, (1024,)), sub(28672, 4096, F32, (1024,))]
    r_VG = [xres("VG0"), xres("VG1")]
    VN = [sub(32768, 2048, BF16, (1024,)), sub(34816, 2048, BF16, (1024,))]
    r_VN = [xres("VN0"), xres("VN1")]
    YAST = sub(36864, 8192, BF16, (8, 512)); r_YAST = xres("YAST")
    VBST = [sub(45056, 2048, BF16, (1024,)), sub(47104, 2048, BF16, (1024,))]
    r_VBST = [xres("VBST0"), xres("VBST1")]
    VCST = sub(49152, 1024, BF16, (4, 128)); r_VCST = xres("VCST")
    stg_i = [0]

    S.op("sp", _dma(ROW, rowtab), writes=[r_ROW], dma=True)
    S.op("sp", _dma(WSF, wsT), writes=[r_WS], dma=True)
    S.op("sp", _dma(TRI, bconst[:, BC_TRI:BC_TRI + 128]), writes=[r_WS], dma=True)
    for g in range(8):
        S.op("dve", _tt(WST[:, g, :], WSF[:, g, :], TRI, ALU.mult), reads=[r_WS], writes=[r_WS])
    qscale = 128.0 ** -0.5

    B.cache_res = Res("wcache"); B.caches = {}; B.uid += 1
    for t in range(B.NT):
        B.cur_t = t
        tsl = slice(t * TT, (t + 1) * TT)
        load_x_tile(B, C, X, r_X, x_in, t)
        make_hn(B, C, X, r_X, HN, r_HN, gcol(l, 2))
        SECT = os.environ.get("MIXA_SECT", "u,v,qb,kb,vb,qc,kcvc").split(",")
        if "u" in SECT:
          fm_group(B, C, w_in, 0, 8, HN, r_HN,
                 lambda j: S.op("act", _act(U[:, j, :], B.bank(j), AF.Gelu), reads=[B.bank_res[j]], writes=[r_U]))
        if "v" in SECT:
            tm_group(B, C, w_in, 1024, HN, r_HN)
        for s in (range(4) if "v" in SECT else []):
            vg, rvg = VG[s % 2], r_VG[s % 2]
            vn, rvn = VN[s % 2], r_VN[s % 2]
            for cb in range(2):
                S.op("act", _act(vg[:, cb * 512:(cb + 1) * 512], B.bank(s * 2 + cb), AF.Gelu),
                     reads=[B.bank_res[s * 2 + cb]], writes=[rvg])
            for cb in range(2):
                S.op("dve", (lambda e, cb=cb, vg=vg: e.bn_stats(LNS[:, cb * 6:(cb + 1) * 6], vg[:, cb * 512:(cb + 1) * 512])),
                     reads=[rvg], writes=[r_LNS])
            S.op("dve", lambda e: e.bn_aggr(LNS[:, 12:14], LNS[:, 0:12]), reads=[r_LNS], writes=[r_LNS])
            S.op("dve", _ts(LNS[:, 13:14], LNS[:, 13:14], LN_EPS, None, ALU.add), reads=[r_LNS], writes=[r_LNS])
            S.op("act", _act(LNS[:, 13:14], LNS[:, 13:14], AF.Sqrt), reads=[r_LNS], writes=[r_LNS])
            S.op("dve", lambda e: e.reciprocal(LNS[:, 13:14], LNS[:, 13:14]), reads=[r_LNS], writes=[r_LNS])
            S.op("dve", _ts(vg, vg, LNS[:, 12:13], LNS[:, 13:14], ALU.subtract, ALU.mult), reads=[rvg, r_LNS], writes=[rvg])
            S.op("dve", _tt(vg, vg, ROW[:, 1, :], ALU.mult), reads=[rvg, r_ROW], writes=[rvg])
            S.op("dve", _tt(vn, vg, ROW[:, 2, :], ALU.add), reads=[rvg, r_ROW], writes=[rvn])
            for g in range(8):
                bk = s * 2 + g // 4
                S.op("pe", _mm(B.bank(bk)[:, (g % 4) * 128:(g % 4 + 1) * 128], vn[:, g * 128:(g + 1) * 128],
                               WST[:, g, :], True, True),
                     reads=[rvn, r_WS], writes=[B.bank_res[bk]])
            for gh in range(2):
                bk = s * 2 + gh
                tm, rtm = next_tmp(C)
                S.op("dve", _tt(tm, B.bank(bk), ROW[:, 0, gh * 512:(gh + 1) * 512], ALU.add),
                     reads=[B.bank_res[bk], r_ROW], writes=[rtm])
                S.op("dve", _tt(YAST[:, gh * 4:(gh + 1) * 4, s * 128:(s + 1) * 128],
                                tm.rearrange("p (a b) -> p a b", a=4),
                                U[:, gh * 4:(gh + 1) * 4, s * 128:(s + 1) * 128], ALU.mult),
                     reads=[rtm, r_U], writes=[r_YAST])
        S.op("sp", _dma(sc["yaT"][:, tsl].rearrange("(g p) t -> p g t", p=128), YAST), reads=[r_YAST],
             writes=[B.dram_res(sc["yaT"])], dma=True)

        def fm_to_scratch(col0, dst, scale, with_km=False):
            i = stg_i[0]; stg_i[0] = 1 - i
            stg, rstg = STG[i], r_STG[i]

            def ev(j):
                if with_km:
                    for hb in range(2):
                        S.op("act", (lambda e, j=j, t=t, hb=hb: e.activation(
                            stg[:, j, hb * 256:(hb + 1) * 256], B.bank(j)[:, hb * 256:(hb + 1) * 256], AF.Copy,
                            accum_out=KM[:, j, t * 2 + hb:t * 2 + hb + 1])),
                             reads=[B.bank_res[j]], writes=[rstg, r_KM])
                elif scale is not None:
                    S.op("dve", _ts(stg[:, j, :], B.bank(j), scale, None, ALU.mult), reads=[B.bank_res[j]], writes=[rstg])
                else:
                    S.op("act", _act(stg[:, j, :], B.bank(j), AF.Copy), reads=[B.bank_res[j]], writes=[rstg])
            fm_group(B, C, w_in, col0, 8, HN, r_HN, ev)
            S.op("sp", _dma(dst[:, tsl].rearrange("(g p) t -> p g t", p=128), stg), reads=[rstg],
                 writes=[B.dram_res(dst)], dma=True)
        if "qb" in SECT:
            fm_to_scratch(2048, sc["qbT"], qscale)
        if "kb" in SECT:
            fm_to_scratch(3072, sc["kbT"], None, with_km=True)
        if "vb" in SECT:
            tm_group(B, C, w_in, 4096, HN, r_HN)
        for s in (range(4) if "vb" in SECT else []):
            vb, rvb = VBST[s % 2], r_VBST[s % 2]
            S.op("act", _act(vb[:, 0:512], B.bank(s * 2), AF.Copy), reads=[B.bank_res[s * 2]], writes=[rvb])
            S.op("dve", _copy(vb[:, 512:1024], B.bank(s * 2 + 1)), reads=[B.bank_res[s * 2 + 1]], writes=[rvb])
            S.op("sp", _dma(sc["vb"][t * TT + s * 128:t * TT + (s + 1) * 128, :], vb), reads=[rvb],
                 writes=[B.dram_res(sc["vb"])], dma=True)
        if "qc" in SECT:
            fm_to_scratch(5120, sc["qcT"], 0.125)
        if "kcvc" not in SECT:
            continue
        i = stg_i[0]; stg_i[0] = 1 - i
        stg, rstg = STG[i], r_STG[i]

        def consume(k, view, sres_slot):
            S.op("pe", _mm(B.bank(0), view(0, 128), HN[:, k, :], k == 0, k == NCH - 1),
                 reads=[sres_slot, r_HN[k]], writes=[B.bank_res[0]])
            for s in range(4):
                S.op("pe", _mm(B.bank(1 + s)[:, 0:128], HN[:, k, s * 128:(s + 1) * 128], view(128, 128),
                               k == 0, k == NCH - 1),
                     reads=[sres_slot, r_HN[k]], writes=[B.bank_res[1 + s]])
        stream_weights(B, C, w_in, 0, NCH, [(6144, 256)], consume)
        S.op("act", _act(stg[:, 0, :], B.bank(0), AF.Copy), reads=[B.bank_res[0]], writes=[rstg])
        S.op("sp", _dma(sc["kcT"][:, tsl], stg[:, 0, :]), reads=[rstg], writes=[B.dram_res(sc["kcT"])], dma=True)
        for s in range(4):
            S.op("dve", _copy(VCST[:, s, :], B.bank(1 + s)[:, 0:128]), reads=[B.bank_res[1 + s]], writes=[r_VCST])
        S.op("sp", _dma(sc["vc"][tsl, :].rearrange("(s p) d -> p s d", p=128), VCST), reads=[r_VCST],
             writes=[B.dram_res(sc["vc"])], dma=True)
    kmf = KM.rearrange("p a b -> p (a b)")
    S.op("dve", _ts(kmf, kmf, 1.0 / 256.0, None, ALU.mult), reads=[r_KM], writes=[r_KM])
    S.op("sp", _dma(sc["kmT"], kmf), reads=[r_KM], writes=[B.dram_res(sc["kmT"])], dma=True)
    B.top = base


def mixB_stage(B, C, l, bconst, fconst, sc, do_moba=True, do_swa=True, NH=8, NQT=4):
    S = B.S
    S.barrier()
    base = B.top
    BC = B.alloc(NBC * 2, BF16, (NBC,)); r_BC = Res("bconst")
    FC = B.alloc(NFC * 4, F32, (NFC,)); r_FC = Res("fconst")
    for c0 in range(0, NBC, 2048):
        c1 = min(NBC, c0 + 2048)
        S.op("sp", _dma(BC[:, c0:c1], bconst[:, c0:c1]), writes=[r_BC], dma=True)
    for c0 in range(0, NFC, 1024):
        c1 = min(NFC, c0 + 1024)
        S.op("sp", _dma(FC[:, c0:c1], fconst[:, c0:c1]), writes=[r_FC], dma=True)
    IDENT = BC[:, BC_IDENT:BC_IDENT + 128]
    PT = [B.alloc(1024, BF16, (512,)) for _ in range(3)]
    r_PT = [Res(f"PT{i}") for i in range(3)]
    YB = B.alloc(4096, BF16, (2048,)); r_YB = Res("YB")
    QT = B.alloc(4096, BF16, (2048,)); r_QT = Res("QT")

    if do_moba:
        KT = B.alloc(16384, BF16, (4, 2048)); r_KT = Res("KT")
        V = B.alloc(16384, BF16, (64, 128)); r_V = Res("V")
        KTo = B.alloc(4096, BF16, (2048,)); r_KTo = Res("KTo")
        Vo = B.alloc(4096, BF16, (16, 128)); r_Vo = Res("Vo")
        KM = B.alloc(64, BF16, (4, 8)); r_KMh = Res("KMh")
        KMF = B.alloc(128, F32, (4, 8)); r_KMF = Res("KMF")
        SELBT = B.alloc(4096, BF16, (2048,)); r_SELBT = Res("SELBT")
        S.op("dve", lambda e: e.memset(SELBT, 0.0), writes=[r_SELBT])
        G = B.alloc(128, F32, (32,)); T8 = B.alloc(32, F32, (8,)); SELB = B.alloc(64, BF16, (32,))
        r_G = Res("G")
        for h in range(NH):
            hs = slice(h * 128, (h + 1) * 128)
            S.op("sp", _dma(KT, sc["kbT_all"].rearrange("(r f) t -> f r t", r=4)[hs, :, :]),
                 reads=[B.dram_res(sc["kbT_all"])], writes=[r_KT], dma=True)
            for r4 in range(4):
                S.op("sp", _dma(V[:, r4 * 16:(r4 + 1) * 16, :],
                                sc["vb_all"][r4 * 2048:(r4 + 1) * 2048, hs].rearrange("(c p) d -> p c d", p=128)),
                     reads=[B.dram_res(sc["vb_all"])], writes=[r_V], dma=True)
            S.op("sp", _dma(KTo, sc["kbT"][hs, :]), reads=[B.dram_res(sc["kbT"])], writes=[r_KTo], dma=True)
            S.op("sp", _dma(Vo, sc["vb"][:, hs].rearrange("(c p) d -> p c d", p=128)),
                 reads=[B.dram_res(sc["vb"])], writes=[r_Vo], dma=True)
            S.op("sp", _dma(QT, sc["qbT"][hs, :]), reads=[B.dram_res(sc["qbT"])], writes=[r_QT], dma=True)
            S.op("sp", _dma(KMF, sc["kmT_all"].rearrange("(r p) c -> p r c", p=128)[:, :, h * 8:(h + 1) * 8]),
                 reads=[B.dram_res(sc["kmT_all"])], writes=[r_KMF], dma=True)
            S.op("dve", _copy(KM, KMF), reads=[r_KMF], writes=[r_KMh])
            for qs in range(4 * NQT):
                gb, gres = B.bank(7), B.bank_res[7]
                S.op("pe", _mm(gb[:, 0:32], QT[:, qs * 128:(qs + 1) * 128], KM.rearrange("p a b -> p (a b)"), True, True),
                     reads=[r_QT, r_KMh], writes=[gres])
                S.op("dve", _tt(G, gb[:, 0:32], FC[:, FC_PASTB + qs * 32:FC_PASTB + (qs + 1) * 32], ALU.add),
                     reads=[gres, r_FC], writes=[r_G])
                S.op("dve", lambda e: e.max(T8, G), reads=[r_G], writes=[r_G])
                S.op("dve", _ts(T8[:, 2:3], T8[:, 2:3], -1e29, None, ALU.max), reads=[r_G], writes=[r_G])
                S.op("dve", _ts(SELB, G, T8[:, 2:3], NEG, ALU.is_lt, ALU.mult), reads=[r_G], writes=[r_G])
                tb = B.bank(6, BF16)
                S.op("pe", (lambda e, tb=tb: e.transpose(tb[0:32, 0:128], SELB, IDENT)), reads=[r_G, r_BC],
                     writes=[B.bank_res[6]])
                S.op("act", _act(SELBT[0:32, qs * 128:(qs + 1) * 128], tb[0:32, 0:128], AF.Copy),
                     reads=[B.bank_res[6]], writes=[r_SELBT])
                if "dbg_g" in sc and h == 0:
                    S.op("sp", _dma(sc["dbg_g"][:, qs * 32:(qs + 1) * 32], G), reads=[r_G], writes=[B.dram_res(sc["dbg_g"])], dma=True)
                    S.op("sp", _dma(sc["dbg_t8"][:, qs * 8:(qs + 1) * 8], T8), reads=[r_G], writes=[B.dram_res(sc["dbg_t8"])], dma=True)
                    S.op("sp", _dma(sc["dbg_selb"][:, qs * 32:(qs + 1) * 32], SELB), reads=[r_G], writes=[B.dram_res(sc["dbg_selb"])], dma=True)
            if "dbg_g" in sc and h == 0:
                S.op("sp", _dma(sc["dbg_selbt"], SELBT), reads=[r_SELBT], writes=[B.dram_res(sc["dbg_selbt"])], dma=True)
            for qt in range(NQT):
                qsl = slice(qt * 512, (qt + 1) * 512)
                chunks = []
                for kc in range(64):
                    chunks.append(("f", KT[:, kc // 16, (kc % 16) * 128:(kc % 16 + 1) * 128], V[:, kc, :], kc // 2,
                                   [r_KT], [r_V]))
                for d4 in range(4):
                    ci = qt * 4 + d4
                    chunks.append(("d", KTo[:, ci * 128:(ci + 1) * 128], Vo[:, ci, :], d4, [r_KTo], [r_Vo]))
                nchk = len(chunks)
                bo, bd = 3, 4

                def qk(i):
                    kind, kap, vap, idx, rk, rv = chunks[i]
                    bs = i % 3
                    S.op("pe", _mm(B.bank(bs), kap, QT[:, qsl], True, False), reads=rk + [r_QT], writes=[B.bank_res[bs]])
                    if kind == "f":
                        S.op("pe", _mm(B.bank(bs), BC[:, BC_EALL + idx * 128:BC_EALL + (idx + 1) * 128],
                                       SELBT[:, qsl], False, True),
                             reads=[r_BC, r_SELBT], writes=[B.bank_res[bs]])
                    else:
                        S.op("pe", _mm(B.bank(bs), IDENT, BC[:, BC_CB + idx * 512:BC_CB + (idx + 1) * 512], False, True),
                             reads=[r_BC], writes=[B.bank_res[bs]])
                    col = FC_ALB + (h * 4 + qt) * 68 + i
                    S.op("act", _act(PT[bs], B.bank(bs), AF.Exp, bias=FC[:, col:col + 1]),
                         reads=[B.bank_res[bs], r_FC], writes=[r_PT[bs]])

                def pv(i):
                    kind, kap, vap, idx, rk, rv = chunks[i]
                    bs = i % 3
                    S.op("pe", _mm(B.bank(bo), vap, PT[bs], i == 0, i == nchk - 1), reads=rv + [r_PT[bs]],
                         writes=[B.bank_res[bo]])
                    S.op("pe", _mm(B.bank(bd), C.ones, PT[bs], i == 0, i == nchk - 1), reads=[r_PT[bs], C.r_const],
                         writes=[B.bank_res[bd]])
                qk(0)
                for i in range(nchk):
                    if i + 1 < nchk:
                        qk(i + 1)
                    pv(i)
                tm, rtm = next_tmp(C)
                S.op("dve", lambda e, tm=tm, bd=bd: e.reciprocal(tm, B.bank(bd)), reads=[B.bank_res[bd]], writes=[rtm])
                S.op("dve", _tt(YB[:, qsl], B.bank(bo), tm, ALU.mult), reads=[B.bank_res[bo], rtm], writes=[r_YB])
            S.op("sp", _dma(sc["ybT"][hs, :], YB), reads=[r_YB], writes=[B.dram_res(sc["ybT"])], dma=True)

    if do_swa:
        KCT = B.alloc(4352, BF16, (2176,)); r_KCT = Res("KCT")
        VCt = B.alloc(4352, BF16, (17, 128)); r_VCt = Res("VCt")
        VPA = B.alloc(4352, BF16, (17, 128)); VPB = B.alloc(4352, BF16, (17, 128)); r_VP = Res("VP")
        HK = B.alloc(1024, BF16, (4, 128)); HV = B.alloc(1024, BF16, (4, 128)); r_H = Res("halo")
        SINKE = B.alloc(32, F32, (8,)); r_SK = Res("sinke")
        TS = B.alloc(2048, F32, (512,)); r_TS = Res("TS")
        S.op("sp", _dma(KCT[:, 128:2176], sc["kcT"]), reads=[B.dram_res(sc["kcT"])], writes=[r_KCT], dma=True)
        S.op("sp", _dma(VCt[:, 1:17, :], sc["vc"].rearrange("(c p) d -> p c d", p=128)),
             reads=[B.dram_res(sc["vc"])], writes=[r_VCt], dma=True)
        S.op("sp", _dma(HK, sc["kcT_all"].rearrange("(r f) t -> f r t", r=4)[:, :, 1920:2048]),
             reads=[B.dram_res(sc["kcT_all"])], writes=[r_H], dma=True)
        S.op("sp", _dma(HV, sc["vc_all"].rearrange("(r c p) d -> p r c d", r=4, p=128)[:, :, 15, :]),
             reads=[B.dram_res(sc["vc_all"])], writes=[r_H], dma=True)
        for (dst, src, rd) in ((KCT[:, 0:128], HK, r_KCT), (VCt[:, 0, :], HV, r_VCt)):
            S.op("dve", _ts(dst, src[:, 0, :], FC[:, FC_HALOW:FC_HALOW + 1], None, ALU.mult), reads=[r_H, r_FC], writes=[rd])
            for r4 in range(1, 4):
                S.op("dve", _stt(dst, src[:, r4, :], FC[:, FC_HALOW + r4:FC_HALOW + r4 + 1], dst, ALU.mult, ALU.add),
                     reads=[r_H, r_FC, rd], writes=[rd])
        SWA_STOP = int(os.environ.get("SWA_STOP", "9"))
        KA = B.alloc(4352, BF16, (2176,)); KB = B.alloc(4352, BF16, (2176,)); r_KAB = Res("KAB")
        S.op("dve", lambda e: e.memset(KA, 0.0), writes=[r_KAB])
        S.op("dve", lambda e: e.memset(KB, 0.0), writes=[r_KAB])
        S.op("dve", _copy(KA[0:64, :], KCT[0:64, :]), reads=[r_KCT], writes=[r_KAB])
        S.op("dve", _copy(KB[64:128, :], KCT[64:128, :]), reads=[r_KCT], writes=[r_KAB])
        S.op("dve", lambda e: e.memset(VPA, 0.0), writes=[r_VP])
        S.op("dve", lambda e: e.memset(VPB, 0.0), writes=[r_VP])
        S.op("dve", _copy(VPA[:, :, 0:64], VCt[:, :, 0:64]), reads=[r_VCt], writes=[r_VP])
        S.op("dve", _copy(VPB[:, :, 64:128], VCt[:, :, 64:128]), reads=[r_VCt], writes=[r_VP])
        S.op("act", _act(SINKE, C.gains[:, SINK0 + l * 8:SINK0 + (l + 1) * 8], AF.Exp), reads=[C.r_gains], writes=[r_SK])
        ONEA = BC[:, BC_ONEA:BC_ONEA + 128]
        ONEB = BC[:, BC_ONEB:BC_ONEB + 128]
        for j in range(8 if SWA_STOP >= 2 else 0):
            S.op("sp", _dma(QT, sc["qcT"][j * 128:(j + 1) * 128, :]), reads=[B.dram_res(sc["qcT"])], writes=[r_QT], dma=True)
            for b in range(4 * NQT if SWA_STOP >= 3 else 0):
                bs = b % 2
                sb, sres = B.bank(bs), B.bank_res[bs]
                bo, bd = 2 + (b // 4) % 2, 4 + (b // 4) % 2
                for hf in range(2):
                    psl = slice(hf * 64, (hf + 1) * 64)
                    for w in range(2):
                        S.op("pe", _mm(sb[:, (hf * 2 + w) * 128:(hf * 2 + w + 1) * 128],
                                       (KA if hf == 0 else KB)[:, (b + w) * 128:(b + w + 1) * 128],
                                       QT[:, b * 128:(b + 1) * 128], True, True),
                             reads=[r_KAB, r_QT], writes=[sres])
                var = 1 if b == 0 else 0
                col = FC_SWAB + (j * 2 + var) * 512
                S.op("dve", _tt(TS, sb, FC[:, col:col + 512], ALU.add), reads=[sres, r_FC], writes=[r_TS])
                pt, rpt = PT[b % 3], r_PT[b % 3]
                S.op("act", _act(pt, TS, AF.Exp), reads=[r_TS], writes=[rpt])
                if SWA_STOP < 4:
                    continue
                osl = slice((b % 4) * 128, (b % 4 + 1) * 128)
                mats = [(VPA[:, b, :], ONEA, 0), (VPA[:, b + 1, :], ONEA, 1), (VPB[:, b, :], ONEB, 2), (VPB[:, b + 1, :], ONEB, 3)]
                for i, (vp, on, c4) in enumerate(mats):
                    S.op("pe", _mm(B.bank(bo)[:, osl], vp, pt[:, c4 * 128:(c4 + 1) * 128], i == 0, i == 3),
                         reads=[r_VP, rpt], writes=[B.bank_res[bo]])
                for i, (vp, on, c4) in enumerate(mats):
                    S.op("pe", _mm(B.bank(bd)[:, osl], on, pt[:, c4 * 128:(c4 + 1) * 128], i == 0, i == 3),
                         reads=[r_BC, rpt], writes=[B.bank_res[bd]])
                if b % 4 == 3:
                    tm, rtm = next_tmp(C)
                    S.op("dve", _ts(tm, B.bank(bd), C_sink(SINKE, j), None, ALU.add), reads=[B.bank_res[bd], r_SK], writes=[rtm])
                    S.op("dve", lambda e, tm=tm: e.reciprocal(tm, tm), reads=[rtm], writes=[rtm])
                    S.op("dve", _tt(YB[:, (b // 4) * 512:(b // 4 + 1) * 512], B.bank(bo), tm, ALU.mult),
                         reads=[B.bank_res[bo], rtm], writes=[r_YB])
            S.op("sp", _dma(sc["ycT"][j * 128:(j + 1) * 128, :], YB), reads=[r_YB], writes=[B.dram_res(sc["ycT"])], dma=True)
    B.top = base


def C_sink(SINKE, j):
    return SINKE[:, j:j + 1]


def mixC_stage(B, C, l, x_in, x_out, yscr, w_gate, w_branch, w_out, sc):
    S = B.S
    S.barrier()
    base = B.top
    X = B.alloc(65536, F32, (32, 512))
    r_X = [Res(f"X{q}") for q in range(4)]
    HN = B.alloc(32768, BF16, (32, 512))
    r_HN = [Res(f"HN{c}") for c in range(NCH)]
    YT = [B.alloc(8192, BF16, (8, 512)) for _ in range(3)]
    r_YT = [Res(f"YT{n}") for n in range(3)]
    SGT = B.alloc(8192, F32, (4, 512)); r_SGT = [Res(f"SGT{j}") for j in range(4)]
    ACC = B.alloc(8192, F32, (4, 512)); r_ACC = [Res(f"ACC{j}") for j in range(4)]
    MERGED = X.rearrange("p a b -> p (a b)")[:, 0:8192].bitcast(BF16).rearrange("p (a b) -> p a b", a=32)
    r_M = [Res(f"M{c}") for c in range(NCH)]
    for c in range(NCH):
        for q in range(4):
            alias(r_M[c], r_X[q])
    ysrc = [sc["yaT"], sc["ybT"], sc["ycT"]]
    B.cache_res = Res("wcache"); B.caches = {}; B.uid += 1
    for t in range(B.NT):
        B.cur_t = t
        tsl = slice(t * TT, (t + 1) * TT)
        load_x_tile(B, C, X, r_X, x_in, t)
        make_hn(B, C, X, r_X, HN, r_HN, gcol(l, 2))
        for n in range(3):
            S.op("sp", _dma(YT[n], ysrc[n][:, tsl].rearrange("(k p) t -> p k t", p=128)),
                 reads=[B.dram_res(ysrc[n])], writes=[r_YT[n]], dma=True)
        for mg in range(8):
            for n in range(3):
                def consume_g(k, view, sres_slot):
                    for j in range(4):
                        S.op("pe", _mm(B.bank(j), view(j * 128, 128), HN[:, k, :], k == 0, k == NCH - 1),
                             reads=[sres_slot, r_HN[k]], writes=[B.bank_res[j]])
                stream_weights(B, C, w_gate, 0, NCH, [(n * D + mg * 512, 512)], consume_g)

                def consume_b(k, view, sres_slot, n=n):
                    for j in range(4):
                        S.op("pe", _mm(B.bank(4 + j), view(j * 128, 128), YT[n][:, k, :], k == 0, k == 7),
                             reads=[sres_slot, r_YT[n]], writes=[B.bank_res[4 + j]])
                stream_weights(B, C, w_branch, n * BW, 8, [(mg * 512, 512)], consume_b)
                for j in range(4):
                    m = mg * 4 + j
                    bcol = BG0 + l * 96 + n * 32 + m
                    S.op("act", _act(SGT[:, j, :], B.bank(j), AF.Sigmoid, bias=C.gains[:, bcol:bcol + 1]),
                         reads=[B.bank_res[j], C.r_gains], writes=[r_SGT[j]])
                    if n == 0:
                        S.op("dve", _tt(ACC[:, j, :], SGT[:, j, :], B.bank(4 + j), ALU.mult),
                             reads=[r_SGT[j], B.bank_res[4 + j]], writes=[r_ACC[j]])
                    else:
                        S.op("dve", _tt(SGT[:, j, :], SGT[:, j, :], B.bank(4 + j), ALU.mult),
                             reads=[r_SGT[j], B.bank_res[4 + j]], writes=[r_SGT[j]])
                        if n == 1:
                            S.op("dve", _tt(ACC[:, j, :], ACC[:, j, :], SGT[:, j, :], ALU.add),
                                 reads=[r_SGT[j], r_ACC[j]], writes=[r_ACC[j]])
                        else:
                            S.op("dve", _tt(MERGED[:, m, :], ACC[:, j, :], SGT[:, j, :], ALU.add),
                                 reads=[r_SGT[j], r_ACC[j]], writes=[r_M[m]])
        out_groups(B, C, w_out, 0, [MERGED[:, k, :] for k in range(NCH)], r_M, yscr)
        residual_tail(B, C, t, x_in, x_out, yscr, gcol(l, 3), False)
    B.top = base


def _slopes():
    i = np.arange(1, 25, dtype=np.float32)
    s = np.exp2(-8.0 * i / 24.0).astype(np.float32)
    return s[:16], s[16:]


_QC_PERM = np.concatenate([np.concatenate([j * 64 + np.arange(64), (8 + j) * 64 + np.arange(64)]) for j in range(8)])


def make_bconst():
    bc = np.zeros((128, NBC), np.float32)
    bc[:, BC_IDENT:BC_IDENT + 128] = np.eye(128)
    p = np.arange(128)
    bc[:, BC_TRI:BC_TRI + 128] = (p[:, None] <= p[None, :])
    for n in range(32):
        bc[n, BC_EALL + n * 128:BC_EALL + (n + 1) * 128] = 1.0
    q = np.arange(512)
    for d4 in range(4):
        ok = ((d4 // 2) == (q[None, :] // 256)) & ((d4 * 128 + p[:, None]) <= q[None, :])
        bc[:, BC_CB + d4 * 512:BC_CB + (d4 + 1) * 512] = np.where(ok, 0.0, NEG)
    bc[:, BC_ONEA:BC_ONEA + 64] = 1.0
    bc[:, BC_ONEB + 64:BC_ONEB + 128] = 1.0
    return bc.astype(ml_dtypes.bfloat16)


def make_fconst(r):
    fc = np.zeros((128, NFC), np.float32)
    swa_s, moba_s = _slopes()
    p = np.arange(128, dtype=np.float32)
    for h in range(8):
        for qt in range(4):
            q0 = r * 2048 + qt * 512
            c0 = FC_ALB + (h * 4 + qt) * 68
            for kc in range(64):
                fc[:, c0 + kc] = moba_s[h] * (kc * 128 + p - q0)
            for d4 in range(4):
                fc[:, c0 + 64 + d4] = moba_s[h] * (d4 * 128 + p)
    for qs in range(16):
        blk = r * 8 + qs // 2
        n = np.arange(32)
        fc[:, FC_PASTB + qs * 32:FC_PASTB + (qs + 1) * 32] = np.where(n < blk, 0.0, -1e30)[None, :]
    s_loc = np.arange(128)[:, None]
    t_loc = np.arange(128)[None, :]
    d_cur = (t_loc - s_loc).astype(np.float32)
    d_prev = (t_loc + 128 - s_loc).astype(np.float32)
    for j in range(8):
        for var in range(2):
            c0 = FC_SWAB + (j * 2 + var) * 512
            for hf, h in enumerate((j, 8 + j)):
                prev = np.where(d_prev < 128, -swa_s[h] * d_prev, -1e30)
                if var == 1 and r == 0:
                    prev = np.full((128, 128), -1e30, np.float32)
                cur = np.where(d_cur >= 0, -swa_s[h] * d_cur, -1e30)
                fc[:, c0 + (hf * 2) * 128:c0 + (hf * 2 + 1) * 128] = prev
                fc[:, c0 + (hf * 2 + 1) * 128:c0 + (hf * 2 + 2) * 128] = cur
    if r > 0:
        fc[:, FC_HALOW + r - 1] = 1.0
    return fc


def pvec(v):
    return np.ascontiguousarray(np.asarray(v, np.float32).reshape(-1, 128).T)


def make_gains(inp):
    g = np.zeros((128, NG), np.float32)
    names = ["ffn1_pre_g", "ffn1_post_g", "mix_pre_g", "mix_post_g", "ffn2_pre_g", "ffn2_post_g"]
    for l in range(2):
        for i, nm in enumerate(names):
            g[:, gcol(l, i):gcol(l, i) + 32] = pvec(inp[nm][l])
        g[:, BG0 + l * 96:BG0 + (l + 1) * 96] = pvec(inp["b_gate"][l])
        sk = np.asarray(inp["swa_sinks"][l], np.float32)
        g[:64, SINK0 + l * 8:SINK0 + (l + 1) * 8] = sk[None, 0:8]
        g[64:, SINK0 + l * 8:SINK0 + (l + 1) * 8] = sk[None, 8:16]
    return g


def layer_arrays(inp, l, skip_ffn=False):
    w_in = np.array(inp["w_in"][l], np.float32, copy=True)
    w_in[:, 5120:6144] = w_in[:, 5120 + _QC_PERM]
    wb = np.array(inp["w_branch"][l], np.float32, copy=True)
    wb[2] = wb[2][_QC_PERM, :]
    a = {} if skip_ffn else {
        f"ffn1_w_up_{l}": inp["ffn1_w_up"][l], f"ffn1_w_down_{l}": inp["ffn1_w_down"][l],
        f"ffn2_w_up_{l}": inp["ffn2_w_up"][l], f"ffn2_w_down_{l}": inp["ffn2_w_down"][l]}
    a.update({
        f"w_in_{l}": w_in, f"w_gate_{l}": inp["w_gate"][l], f"w_branch_{l}": wb.reshape(3 * BW, D),
        f"w_out_{l}": inp["w_out"][l],
        f"wsT_{l}": np.ascontiguousarray(np.asarray(inp["gmlp_w_s"][l], np.float32).transpose(2, 0, 1)),
        f"rowtab_{l}": np.ascontiguousarray(np.broadcast_to(np.stack([
            np.asarray(inp["gmlp_b_s"][l], np.float32).reshape(1024),
            np.asarray(inp["gmlp_ln_g"][l], np.float32), np.asarray(inp["gmlp_ln_b"][l], np.float32)])[None], (128, 3, 1024))),
    })
    return {k: np.ascontiguousarray(np.asarray(v, np.float32)) for k, v in a.items()}


SCRATCH = {
    "qbT": ([BW, TOK], BF16), "kbT": ([BW, TOK], BF16), "vb": ([TOK, BW], BF16), "kmT": ([128, 64], F32),
    "qcT": ([BW, TOK], BF16), "kcT": ([128, TOK], BF16), "vc": ([TOK, 128], BF16), "yaT": ([BW, TOK], BF16),
    "ybT": ([BW, TOK], BF16), "ycT": ([BW, TOK], BF16),
    "kbT_all": ([4 * BW, TOK], BF16), "vb_all": ([4 * TOK, BW], BF16), "kmT_all": ([512, 64], F32),
    "kcT_all": ([4 * 128, TOK], BF16), "vc_all": ([4 * TOK, 128], BF16),
}
WSHAPES = {"ffn1_w_up": [D, 2 * DFF], "ffn1_w_down": [DFF, D], "ffn2_w_up": [D, 2 * DFF], "ffn2_w_down": [DFF, D],
           "w_in": [D, IN_COLS], "w_gate": [D, 3 * D], "w_branch": [3 * BW, D], "w_out": [D, D],
           "wsT": [128, 8, 128], "rowtab": [128, 3, 1024]}


def build_launch(stages, ext_in, ext_out, NT=4, mixb_kw=None):
    def stage_fn(nc, B):
        made = {}

        def T(name, shape, dtype):
            if name not in made:
                kind = "ExternalInput" if name in ext_in else ("ExternalOutput" if name in ext_out else "Internal")
                made[name] = nc.dram_tensor(name, shape, dtype, kind=kind).ap()
            return made[name]

        def W(nm, l):
            return T(f"{nm}_{l}", WSHAPES[nm], F32)

        def SC(l):
            d = {k: T(f"{k}_{l}", sh, dt) for k, (sh, dt) in SCRATCH.items()}
            if os.environ.get("MIXB_DBG"):
                d["dbg_g"] = T("dbg_g", [128, 512], F32); d["dbg_t8"] = T("dbg_t8", [128, 128], F32)
                d["dbg_selb"] = T("dbg_selb", [128, 512], BF16); d["dbg_selbt"] = T("dbg_selbt", [128, 2048], BF16)
            return d
        bconst = T("bconst", [128, NBC], BF16)
        fconst = T("fconst", [128, NFC], F32)
        gains = T("gains", [128, NG], F32)
        C = setup_common(B, bconst, gains)
        yscr = T("yscr", [D, TT], F32)
        outs = []
        for st in stages:
            if st[0] == "ffn":
                _, l, which, xi, xo = st
                pre, post = (0, 1) if which == 1 else (4, 5)
                ffn_stage(B, C, T(xi, [D, TOK], F32), T(xo, [D, TOK], F32), yscr,
                          W(f"ffn{which}_w_up", l), W(f"ffn{which}_w_down", l), gcol(l, pre), gcol(l, post))
            elif st[0] == "mixA":
                _, l, xi = st
                mixA_stage(B, C, l, T(xi, [D, TOK], F32), W("w_in", l), W("wsT", l), W("rowtab", l), bconst, SC(l))
            elif st[0] == "mixB":
                _, l = st
                mixB_stage(B, C, l, bconst, fconst, SC(l), **(mixb_kw or {}))
            elif st[0] == "mixC":
                _, l, xi, xo = st
                mixC_stage(B, C, l, T(xi, [D, TOK], F32), T(xo, [D, TOK], F32), yscr,
                           W("w_gate", l), W("w_branch", l), W("w_out", l), SC(l))
        final_wait(B, [B.dram_res(made[n]) for n in made if n in ext_out])
    return make_program(stage_fn, NT=NT)


OWN = ["qbT", "kbT", "vb", "kmT", "qcT", "kcT", "vc", "yaT"]
GATH = ["kbT", "vb", "kmT", "kcT", "vc"]


def run_launch(nc, in_maps, n_cores=8):
    res = run_bass_kernel_spmd(nc, in_maps, core_ids=list(range(n_cores)))
    return res.results


def gather_maps(outs, l, n_cores=8):
    g = []
    for c in range(n_cores):
        b0 = (c // 4) * 4
        d = {}
        for k in GATH:
            d[f"{k}_all_{l}"] = np.concatenate([outs[b0 + r][f"{k}_{l}"] for r in range(4)], axis=0)
        g.append(d)
    return g


def kernel(**inp):
    inp = {k: np.asarray(v) for k, v in inp.items()}
    n = 8
    bconst = make_bconst()
    gains = make_gains(inp)
    fcs = [make_fconst(c % 4) for c in range(n)]
    LA = [layer_arrays(inp, 0), layer_arrays(inp, 1)]
    x = inp["x"]
    base = [{"bconst": bconst, "gains": gains, "fconst": fcs[c]} for c in range(n)]
    consts = {"bconst", "gains", "fconst"}
    own0 = {f"{k}_0" for k in OWN}
    own1 = {f"{k}_1" for k in OWN}
    all0 = {f"{k}_all_0" for k in GATH}
    all1 = {f"{k}_all_1" for k in GATH}

    def wnames(l, names):
        return {f"{nm}_{l}" for nm in names}

    w1 = wnames(0, ["ffn1_w_up", "ffn1_w_down", "w_in", "wsT", "rowtab"])
    nc1 = build_launch([("ffn", 0, 1, "x0", "x1"), ("mixA", 0, "x1")], consts | {"x0"} | w1, {"x1"} | own0)
    maps = []
    for c in range(n):
        m = dict(base[c])
        m["x0"] = np.ascontiguousarray(x[c // 4, (c % 4) * TOK:(c % 4 + 1) * TOK, :].T)
        m.update({k: LA[0][k] for k in w1})
        maps.append(m)
    o1 = run_launch(nc1, maps)
    del maps
    w2 = wnames(0, ["w_gate", "w_branch", "w_out", "ffn2_w_up", "ffn2_w_down"]) | \
        wnames(1, ["ffn1_w_up", "ffn1_w_down", "w_in", "wsT", "rowtab"])
    nc2 = build_launch([("mixB", 0), ("mixC", 0, "x1", "x2"), ("ffn", 0, 2, "x2", "x3"), ("ffn", 1, 1, "x3", "x4"),
                        ("mixA", 1, "x4")], consts | {"x1"} | own0 | all0 | w2, {"x4"} | own1)
    g1 = gather_maps(o1, 0)
    maps = []
    for c in range(n):
        m = dict(base[c])
        m["x1"] = o1[c]["x1"]
        m.update({k: o1[c][k] for k in own0})
        m.update(g1[c])
        for k in w2:
            m[k] = LA[int(k[-1])][k]
        maps.append(m)
    o2 = run_launch(nc2, maps)
    del maps, o1, g1
    w3 = wnames(1, ["w_gate", "w_branch", "w_out", "ffn2_w_up", "ffn2_w_down"])
    nc3 = build_launch([("mixB", 1), ("mixC", 1, "x4", "x5"), ("ffn", 1, 2, "x5", "xout")],
                       consts | {"x4"} | own1 | all1 | w3, {"xout"})
    g2 = gather_maps(o2, 1)
    maps = []
    for c in range(n):
        m = dict(base[c])
        m["x4"] = o2[c]["x4"]
        m.update({k: o2[c][k] for k in own1})
        m.update(g2[c])
        m.update({k: LA[1][k] for k in w3})
        maps.append(m)
    o3 = run_launch(nc3, maps)
    out = np.empty_like(x)
    for c in range(n):
        out[c // 4, (c % 4) * TOK:(c % 4 + 1) * TOK, :] = o3[c]["xout"].T
    return out
```

```python
import contextlib
import os
import numpy as np
import ml_dtypes
import concourse.bass as bass
import concourse.mybir as mybir
from concourse.bass_utils import run_bass_kernel_spmd

F32 = mybir.dt.float32
BF16 = mybir.dt.bfloat16
AF = mybir.ActivationFunctionType
ALU = mybir.AluOpType
AX = mybir.AxisListType

D = 4096
NCH = D // 128
TT = 512
TOK = 2048
DFF = 8192
BW = 1024
IN_COLS = 6400
RMS_EPS = 1e-6
LN_EPS = 1e-5
NEG = -30000.0
SEM_ROT = 28000
SAME_ENGINE_SYNC = True
NSLOT = 5
WCACHE = True


class Res:
    __slots__ = ("name", "last_write", "readers", "dma_readers", "aliases", "sem", "cnt")

    def __init__(self, name):
        self.name = name
        self.last_write = None
        self.readers = {}
        self.dma_readers = []
        self.aliases = []
        self.sem = None
        self.cnt = 0


def alias(a, b):
    a.aliases.append(b)
    b.aliases.append(a)


class Op:
    __slots__ = ("eng", "fn", "deps", "tick", "token", "is_dma", "dres")

    def __init__(self, eng, fn, is_dma, dres):
        self.eng = eng
        self.fn = fn
        self.deps = []
        self.tick = False
        self.token = None
        self.is_dma = is_dma
        self.dres = dres


class Sched:
    ENGS = ("pe", "act", "dve", "pool", "sp")

    def __init__(self, sem_pool):
        self.sem_pool = list(sem_pool)
        self.ops = []
        self.by_eng = {e: [] for e in self.ENGS}
        self.pending_barrier = {e: None for e in self.ENGS}
        self.dma_since_barrier = []
        self.last_op = {e: None for e in self.ENGS}

    def op(self, eng, fn, reads=(), writes=(), dma=False):
        o = Op(eng, fn, dma, writes[0] if dma else None)
        deps = []
        for r in reads:
            if r.last_write is not None:
                deps.append(r.last_write)
            for a in r.aliases:
                if a.last_write is not None:
                    deps.append(a.last_write)
        for w in writes:
            for x in [w] + w.aliases:
                if x.last_write is not None:
                    deps.append(x.last_write)
                deps.extend(x.readers.values())
                deps.extend(x.dma_readers)
        pb = self.pending_barrier[eng]
        if pb is not None:
            deps.extend(pb)
            self.pending_barrier[eng] = None
        seen = set()
        for d in deps:
            if d is o or id(d) in seen:
                continue
            seen.add(id(d))
            if (not d.is_dma) and (not dma) and d.eng == eng:
                if eng == "pe" or not SAME_ENGINE_SYNC:
                    continue
            d.tick = True
            o.deps.append(d)
        for r in reads:
            if dma:
                r.dma_readers.append(o)
            else:
                r.readers[eng] = o
        for w in writes:
            w.last_write = o
            w.readers = {}
            w.dma_readers = []
        self.ops.append(o)
        self.by_eng[eng].append(o)
        self.last_op[eng] = o
        if dma:
            self.dma_since_barrier.append(o)
        return o

    def barrier(self):
        deps = [o for o in self.last_op.values() if o is not None and not o.is_dma]
        deps += self.dma_since_barrier
        self.dma_since_barrier = []
        for e in self.ENGS:
            old = self.pending_barrier[e] or []
            self.pending_barrier[e] = old + deps

    def number(self):
        eng_sem = {}
        eng_cnt = {}
        for o in self.ops:
            if o.is_dma:
                r = o.dres
                if r.sem is None or r.cnt >= SEM_ROT:
                    r.sem = self.sem_pool.pop()
                    r.cnt = 0
                r.cnt += 16
                o.token = (r.sem, r.cnt)
            elif o.tick:
                e = o.eng
                if e not in eng_sem or eng_cnt[e] >= SEM_ROT:
                    eng_sem[e] = self.sem_pool.pop()
                    eng_cnt[e] = 0
                eng_cnt[e] += 1
                o.token = (eng_sem[e], eng_cnt[e])

    def emit(self, eng_name, eng):
        waited = {}
        for o in self.by_eng[eng_name]:
            for d in o.deps:
                sem, val = d.token
                k = id(sem)
                if waited.get(k, (None, 0))[1] < val:
                    eng.wait_ge(sem, val)
                    waited[k] = (sem, val)
            ins = o.fn(eng)
            if o.token is not None:
                ins.then_inc(o.token[0], 16 if o.is_dma else 1)


class Builder:
    def __init__(self, nc, S, arena, ps, NT):
        self.nc = nc
        self.S = S
        self.arena = arena
        self.ps = ps
        self.NT = NT
        self.top = 0
        self.bank_res = [Res(f"bank{i}") for i in range(8)]
        self.dram = {}
        self.caches = {}
        self.cache_res = None
        self.cur_t = 0
        self.uid = 0

    def alloc(self, nbytes, dtype, shape_free):
        assert nbytes % 4 == 0
        off = self.top // 4
        self.top += nbytes
        assert self.top <= self.arena_bytes, (self.top, self.arena_bytes)
        ap = self.arena[:, off:off + nbytes // 4]
        if dtype != F32:
            ap = ap.bitcast(dtype)
        if len(shape_free) == 2:
            ap = ap.rearrange("p (a b) -> p a b", a=shape_free[0])
        elif len(shape_free) == 3:
            ap = ap.rearrange("p (a b c) -> p a b c", a=shape_free[0], b=shape_free[1])
        return ap

    def bank(self, i, dtype=F32):
        ap = self.ps[:, i * 512:(i + 1) * 512]
        if dtype != F32:
            ap = ap.bitcast(dtype)
        return ap


def _mm(out, lhsT, rhs, start, stop):
    return lambda e: e.matmul(out, lhsT, rhs, start=start, stop=stop)


def _act(out, in_, func, bias=None, scale=None):
    kw = {}
    if bias is not None:
        kw["bias"] = bias
    if scale is not None:
        kw["scale"] = scale
    return lambda e: e.activation(out, in_, func, **kw)


def _dma(out, in_):
    return lambda e: e.dma_start(out=out, in_=in_)


def _ts(out, in0, s1, s2, op0, op1=None):
    if op1 is None:
        return lambda e: e.tensor_scalar(out, in0, s1, None, op0)
    return lambda e: e.tensor_scalar(out, in0, s1, s2, op0, op1)


def _tt(out, in0, in1, op):
    return lambda e: e.tensor_tensor(out, in0, in1, op)


def _stt(out, in0, scalar, in1, op0, op1):
    return lambda e: e.scalar_tensor_tensor(out, in0, scalar, in1, op0, op1)


def _copy(out, in_):
    return lambda e: e.tensor_copy(out, in_)


class Common:
    pass


def setup_common(B, consts_ap, gains_ap):
    S = B.S
    C = Common()
    C.ones = B.alloc(256, BF16, (128,))
    C.ident = B.alloc(256, BF16, (128,))
    C.r_const = Res("const")
    S.op("sp", _dma(C.ident, consts_ap[:, 0:128]), writes=[C.r_const], dma=True)
    S.op("dve", lambda e: e.memset(C.ones, 1.0), writes=[C.r_const])
    ng = gains_ap.shape[1]
    C.gains = B.alloc(ng * 4, F32, (ng,))
    C.r_gains = Res("gains")
    S.op("sp", _dma(C.gains, gains_ap), writes=[C.r_gains], dma=True)
    C.slots = [B.alloc(8192, BF16, (4, 1024)) for _ in range(NSLOT)]
    C.slot_res = [Res(f"slot{i}") for i in range(NSLOT)]
    C.slot_i = 0
    C.rstd = [B.alloc(2048, F32, (512,)) for _ in range(2)]
    C.r_rstd = [Res("rstd0"), Res("rstd1")]
    C.sq = [B.alloc(1024, BF16, (512,)) for _ in range(2)]
    C.r_sq = [Res("sq0"), Res("sq1")]
    C.tmp = [B.alloc(2048, F32, (512,)) for _ in range(4)]
    C.r_tmp = [Res(f"tmp{i}") for i in range(4)]
    C.tmp_i = 0
    return C


def next_tmp(C):
    i = C.tmp_i
    C.tmp_i = (i + 1) % 4
    return C.tmp[i], C.r_tmp[i]


def stream_weights(B, C, w_ap, row0, nk, col_ranges, consume):
    S = B.S
    nq = (nk + 3) // 4
    W = sum(n for _, n in col_ranges)
    use_cache = WCACHE and B.NT > 1 and getattr(B, "cache_res", None) is not None
    cache = None
    if use_cache:
        key = (w_ap.tensor.name, row0, tuple(col_ranges))
        if key not in B.caches:
            B.caches[key] = B.nc.dram_tensor(f"wc{len(B.caches)}_{B.uid}", [nq * 128, 4 * W], BF16, kind="Internal").ap()
        cache = B.caches[key]
    for q in range(nq):
        si = C.slot_i
        C.slot_i = (si + 1) % NSLOT
        slot = C.slots[si].rearrange("p a b -> p (a b)")
        sres = C.slot_res[si]
        kc_n = min(4, nk - q * 4)
        flat = slot[:, 0:kc_n * W]
        s3 = flat.rearrange("p (kc c) -> p kc c", kc=kc_n)
        if use_cache and B.cur_t > 0:
            S.op("pool", _dma(flat, cache[q * 128:(q + 1) * 128, 0:kc_n * W]), reads=[B.cache_res],
                 writes=[sres], dma=True)
        else:
            off = 0
            for (c0, n) in col_ranges:
                src = w_ap[row0 + q * 512: row0 + q * 512 + kc_n * 128, c0:c0 + n].rearrange(
                    "(kc p) c -> p kc c", p=128)
                S.op("pool", _dma(s3[:, :, off:off + n], src), writes=[sres], dma=True)
                off += n
            if use_cache:
                S.op("sp", _dma(cache[q * 128:(q + 1) * 128, 0:kc_n * W], flat), reads=[sres], writes=[B.cache_res],
                     dma=True)
        for kc in range(kc_n):
            k = q * 4 + kc
            consume(k, (lambda o, n, _s=slot, _kc=kc: _s[:, _kc * W + o:_kc * W + o + n]), sres)


def rms_stats(B, C, chunks, chunk_res, which, eps, bank_i):
    S = B.S
    bank = B.bank(bank_i)
    bres = B.bank_res[bank_i]
    n = len(chunks)
    for c in range(n):
        sq, rsq = C.sq[c % 2], C.r_sq[c % 2]
        S.op("act", _act(sq, chunks[c], AF.Square), reads=[chunk_res[c]], writes=[rsq])
        S.op("pe", _mm(bank, C.ones, sq, c == 0, c == n - 1), reads=[rsq, C.r_const], writes=[bres])
    rstd, rr = C.rstd[which], C.r_rstd[which]
    S.op("dve", _ts(rstd, bank, 1.0 / D, eps, ALU.mult, ALU.add), reads=[bres], writes=[rr])
    S.op("act", _act(rstd, rstd, AF.Sqrt), reads=[rr], writes=[rr])
    S.op("dve", lambda e: e.reciprocal(rstd, rstd), reads=[rr], writes=[rr])
    return rstd, rr


def load_x_tile(B, C, X, r_X, x_in, t):
    S = B.S
    for q in range(4):
        src = x_in[q * 1024:(q + 1) * 1024, t * TT:(t + 1) * TT].rearrange("(c p) t -> p c t", p=128)
        S.op("sp", _dma(X[:, q * 8:(q + 1) * 8, :], src), reads=[B.dram_res(x_in)], writes=[r_X[q]], dma=True)


def make_hn(B, C, X, r_X, HN, r_HN, gcol):
    S = B.S
    rstd, rr = rms_stats(B, C, [X[:, c, :] for c in range(NCH)], [r_X[c // 8] for c in range(NCH)], 0, RMS_EPS, 0)
    for c in range(NCH):
        S.op("dve", _stt(HN[:, c, :], X[:, c, :], C.gains[:, gcol + c:gcol + c + 1], rstd, ALU.mult, ALU.mult),
             reads=[r_X[c // 8], rr, C.r_gains], writes=[r_HN[c]])


def residual_tail(B, C, t, x_in, x_out, yscr, gcol, half):
    S = B.S
    rstd, rr = C.rstd[1], C.r_rstd[1]
    for c in range(NCH):
        ty, rty = next_tmp(C)
        tx, rtx = next_tmp(C)
        S.op("sp", _dma(ty, yscr[c * 128:(c + 1) * 128, :]), reads=[B.dram_res(yscr)], writes=[rty], dma=True)
        S.op("sp", _dma(tx, x_in[c * 128:(c + 1) * 128, t * TT:(t + 1) * TT]), reads=[B.dram_res(x_in)],
             writes=[rtx], dma=True)
        S.op("dve", _stt(ty, ty, C.gains[:, gcol + c:gcol + c + 1], rstd, ALU.mult, ALU.mult),
             reads=[rty, rr, C.r_gains], writes=[rty])
        if half:
            S.op("dve", _stt(tx, ty, 0.5, tx, ALU.mult, ALU.add), reads=[rty, rtx], writes=[rtx])
        else:
            S.op("dve", _tt(tx, ty, tx, ALU.add), reads=[rty, rtx], writes=[rtx])
        S.op("sp", _dma(x_out[c * 128:(c + 1) * 128, t * TT:(t + 1) * TT], tx), reads=[rtx],
             writes=[B.dram_res(x_out)], dma=True)


def out_groups(B, C, w_ap, row0, rhs_chunks, rhs_res, yscr, evac_extra=None):
    S = B.S
    nk = len(rhs_chunks)
    sbank, sres = B.bank(7), B.bank_res[7]
    groups = [list(range(i, min(i + 7, NCH))) for i in range(0, NCH, 7)]
    done = 0
    for grp in groups:
        c0 = grp[0] * 128
        ncols = len(grp) * 128

        def consume(k, view, sres_slot, grp=grp):
            for j, m in enumerate(grp):
                S.op("pe", _mm(B.bank(j), view(j * 128, 128), rhs_chunks[k], k == 0, k == nk - 1),
                     reads=[sres_slot, rhs_res[k]], writes=[B.bank_res[j]])
        stream_weights(B, C, w_ap, row0, nk, [(c0, ncols)], consume)
        for j, m in enumerate(grp):
            ty, rty = next_tmp(C)
            eng = "act" if (j % 2 == 0) else "dve"
            if eng == "act":
                S.op("act", _act(ty, B.bank(j), AF.Copy), reads=[B.bank_res[j]], writes=[rty])
            else:
                S.op("dve", _copy(ty, B.bank(j)), reads=[B.bank_res[j]], writes=[rty])
            sq, rsq = C.sq[done % 2], C.r_sq[done % 2]
            S.op("act", _act(sq, ty, AF.Square), reads=[rty], writes=[rsq])
            S.op("pe", _mm(sbank, C.ones, sq, done == 0, done == NCH - 1), reads=[rsq, C.r_const], writes=[sres])
            S.op("sp", _dma(yscr[m * 128:(m + 1) * 128, :], ty), reads=[rty], writes=[B.dram_res(yscr)], dma=True)
            done += 1
    rstd, rr = C.rstd[1], C.r_rstd[1]
    S.op("dve", _ts(rstd, sbank, 1.0 / D, RMS_EPS, ALU.mult, ALU.add), reads=[sres], writes=[rr])
    S.op("act", _act(rstd, rstd, AF.Sqrt), reads=[rr], writes=[rr])
    S.op("dve", lambda e: e.reciprocal(rstd, rstd), reads=[rr], writes=[rr])


def ffn_stage(B, C, x_in, x_out, yscr, w_up, w_down, g_pre_col, g_post_col):
    S = B.S
    S.barrier()
    base = B.top
    X = B.alloc(65536, F32, (32, 512))
    r_X = [Res(f"X{q}") for q in range(4)]
    HN = B.alloc(32768, BF16, (32, 512))
    r_HN = [Res(f"HN{c}") for c in range(NCH)]
    ACTB = X.rearrange("p a b -> p (a b)").bitcast(BF16).rearrange("p (a b) -> p a b", a=64)
    r_A = [Res(f"A{j}") for j in range(64)]
    for j in range(64):
        alias(r_A[j], r_X[j // 16])
    B.cache_res = Res("wcache"); B.caches = {}; B.uid += 1
    for t in range(B.NT):
        B.cur_t = t
        if t == 0:
            load_x_tile(B, C, X, r_X, x_in, t)
        make_hn(B, C, X, r_X, HN, r_HN, g_pre_col)
        for g in range(16):
            def consume(k, view, sres_slot):
                for j in range(8):
                    S.op("pe", _mm(B.bank(j), view(j * 128, 128), HN[:, k, :], k == 0, k == NCH - 1),
                         reads=[sres_slot, r_HN[k]], writes=[B.bank_res[j]])
            stream_weights(B, C, w_up, 0, NCH, [(g * 512, 512), (DFF + g * 512, 512)], consume)
            for j in range(4):
                ty, rty = next_tmp(C)
                S.op("act", _act(ty, B.bank(j), AF.Silu), reads=[B.bank_res[j]], writes=[rty])
                S.op("dve", _tt(ACTB[:, g * 4 + j, :], ty, B.bank(4 + j), ALU.mult),
                     reads=[rty, B.bank_res[4 + j]], writes=[r_A[g * 4 + j]])
        out_groups(B, C, w_down, 0, [ACTB[:, k, :] for k in range(64)], r_A, yscr)
        if t + 1 < B.NT:
            load_x_tile(B, C, X, r_X, x_in, t + 1)
        residual_tail(B, C, t, x_in, x_out, yscr, g_post_col, True)
    B.top = base


def _builder_extras():
    def dram_res(self, ap):
        name = ap.tensor.name if hasattr(ap, "tensor") else ap.name
        if name not in self.dram:
            self.dram[name] = Res("dram_" + name)
        return self.dram[name]
    Builder.dram_res = dram_res


_builder_extras()

ARENA_BYTES = 204 * 1024


def make_program(stage_fn, NT=4):
    nc = bass.Bass("TRN2", target_bir_lowering=False)
    with contextlib.ExitStack() as es:
        arena = es.enter_context(nc.sbuf_tensor("arena", [128, ARENA_BYTES // 4], F32))
        ps = es.enter_context(nc.psum_tensor("ps", [128, 4096], F32))
        sems = [es.enter_context(nc.semaphore(f"s{i}")) for i in range(90)]
        S = Sched(sems)
        B = Builder(nc, S, arena, ps, NT)
        B.arena_bytes = ARENA_BYTES
        stage_fn(nc, B)
        S.number()
        block = es.enter_context(nc.Block())

        @block.tensor
        def _(e):
            S.emit("pe", e)

        @block.scalar
        def _(e):
            S.emit("act", e)

        @block.vector
        def _(e):
            S.emit("dve", e)

        @block.gpsimd
        def _(e):
            S.emit("pool", e)

        @block.sync
        def _(e):
            S.emit("sp", e)
    return nc


def final_wait(B, out_res_list):
    B.S.op("sp", lambda e: None, reads=out_res_list)


def gcol(l, i):
    return (l * 6 + i) * 32


BG0 = 2 * 6 * 32
SINK0 = BG0 + 2 * 96
NG = SINK0 + 16

BC_IDENT = 0
BC_TRI = 128
BC_EALL = 256
BC_CB = BC_EALL + 4096
BC_ONEA = BC_CB + 2048
BC_ONEB = BC_ONEA + 128
NBC = BC_ONEB + 128
FC_ALB = 0
FC_PASTB = FC_ALB + 8 * 4 * 68
FC_SWAB = FC_PASTB + 512
FC_HALOW = FC_SWAB + 8 * 2 * 512
NFC = FC_HALOW + 4


def fm_group(B, C, w_ap, col0, nchunks, HN, r_HN, evac):
    S = B.S

    def consume(k, view, sres_slot):
        for j in range(nchunks):
            S.op("pe", _mm(B.bank(j), view(j * 128, 128), HN[:, k, :], k == 0, k == NCH - 1),
                 reads=[sres_slot, r_HN[k]], writes=[B.bank_res[j]])
    nc_ = nchunks * 128
    rngs = [(col0, nc_)] if nc_ <= 512 else [(col0, 512), (col0 + 512, nc_ - 512)]
    stream_weights(B, C, w_ap, 0, NCH, rngs, consume)
    for j in range(nchunks):
        evac(j)


def tm_group(B, C, w_ap, col0, HN, r_HN):
    S = B.S

    def consume(k, view, sres_slot):
        for s in range(4):
            for cb in range(2):
                S.op("pe", _mm(B.bank(s * 2 + cb), HN[:, k, s * 128:(s + 1) * 128], view(cb * 512, 512),
                               k == 0, k == NCH - 1),
                     reads=[sres_slot, r_HN[k]], writes=[B.bank_res[s * 2 + cb]])
    stream_weights(B, C, w_ap, 0, NCH, [(col0, 512), (col0 + 512, 512)], consume)


def mixA_stage(B, C, l, x_in, w_in, wsT, rowtab, bconst, sc):
    S = B.S
    S.barrier()
    base = B.top
    X = B.alloc(65536, F32, (32, 512))
    r_X = [Res(f"X{q}") for q in range(4)]
    HN = B.alloc(32768, BF16, (32, 512))
    r_HN = [Res(f"HN{c}") for c in range(NCH)]
    ROW = B.alloc(12288, F32, (3, 1024))
    r_ROW = Res("rowtab")
    WSF = B.alloc(4096, F32, (8, 128))
    WST = B.alloc(2048, BF16, (8, 128))
    TRI = B.alloc(256, BF16, (128,))
    r_WS = Res("wst")
    LNS = B.alloc(64, F32, (16,))
    r_LNS = Res("lns")
    KM = B.alloc(256, F32, (8, 8))
    r_KM = Res("km")
    xb = X.rearrange("p a b -> p (a b)")

    def sub(off, nbytes, dtype, shape):
        ap = xb[:, off // 4:(off + nbytes) // 4]
        if dtype != F32:
            ap = ap.bitcast(dtype)
        if len(shape) == 2:
            ap = ap.rearrange("p (a b) -> p a b", a=shape[0])
        return ap

    def xres(name):
        r = Res(name)
        for q in range(4):
            alias(r, r_X[q])
        return r
    U = sub(0, 8192, BF16, (8, 512)); r_U = xres("U")
    STG = [sub(8192, 8192, BF16, (8, 512)), sub(16384, 8192, BF16, (8, 512))]
    r_STG = [xres("STG0"), xres("STG1")]
    VG = [sub(24576, 4096, F32, (1024,)), sub(28672, 4096, F32, (1024,))]
    r_VG = [xres("VG0"), xres("VG1")]
    VN = [sub(32768, 2048, BF16, (1024,)), sub(34816, 2048, BF16, (1024,))]
    r_VN = [xres("VN0"), xres("VN1")]
    YAST = sub(36864, 8192, BF16, (8, 512)); r_YAST = xres("YAST")
    VBST = [sub(45056, 2048, BF16, (1024,)), sub(47104, 2048, BF16, (1024,))]
    r_VBST = [xres("VBST0"), xres("VBST1")]
    VCST = sub(49152, 1024, BF16, (4, 128)); r_VCST = xres("VCST")
    stg_i = [0]

    S.op("sp", _dma(ROW, rowtab), writes=[r_ROW], dma=True)
    S.op("sp", _dma(WSF, wsT), writes=[r_WS], dma=True)
    S.op("sp", _dma(TRI, bconst[:, BC_TRI:BC_TRI + 128]), writes=[r_WS], dma=True)
    for g in range(8):
        S.op("dve", _tt(WST[:, g, :], WSF[:, g, :], TRI, ALU.mult), reads=[r_WS], writes=[r_WS])
    qscale = 128.0 ** -0.5

    B.cache_res = Res("wcache"); B.caches = {}; B.uid += 1
    for t in range(B.NT):
        B.cur_t = t
        tsl = slice(t * TT, (t + 1) * TT)
        load_x_tile(B, C, X, r_X, x_in, t)
        make_hn(B, C, X, r_X, HN, r_HN, gcol(l, 2))
        SECT = os.environ.get("MIXA_SECT", "u,v,qb,kb,vb,qc,kcvc").split(",")
        if "u" in SECT:
          fm_group(B, C, w_in, 0, 8, HN, r_HN,
                 lambda j: S.op("act", _act(U[:, j, :], B.bank(j), AF.Gelu), reads=[B.bank_res[j]], writes=[r_U]))
        if "v" in SECT:
            tm_group(B, C, w_in, 1024, HN, r_HN)
        for s in (range(4) if "v" in SECT else []):
            vg, rvg = VG[s % 2], r_VG[s % 2]
            vn, rvn = VN[s % 2], r_VN[s % 2]
            for cb in range(2):
                S.op("act", _act(vg[:, cb * 512:(cb + 1) * 512], B.bank(s * 2 + cb), AF.Gelu),
                     reads=[B.bank_res[s * 2 + cb]], writes=[rvg])
            for cb in range(2):
                S.op("dve", (lambda e, cb=cb, vg=vg: e.bn_stats(LNS[:, cb * 6:(cb + 1) * 6], vg[:, cb * 512:(cb + 1) * 512])),
                     reads=[rvg], writes=[r_LNS])
            S.op("dve", lambda e: e.bn_aggr(LNS[:, 12:14], LNS[:, 0:12]), reads=[r_LNS], writes=[r_LNS])
            S.op("dve", _ts(LNS[:, 13:14], LNS[:, 13:14], LN_EPS, None, ALU.add), reads=[r_LNS], writes=[r_LNS])
            S.op("act", _act(LNS[:, 13:14], LNS[:, 13:14], AF.Sqrt), reads=[r_LNS], writes=[r_LNS])
            S.op("dve", lambda e: e.reciprocal(LNS[:, 13:14], LNS[:, 13:14]), reads=[r_LNS], writes=[r_LNS])
            S.op("dve", _ts(vg, vg, LNS[:, 12:13], LNS[:, 13:14], ALU.subtract, ALU.mult), reads=[rvg, r_LNS], writes=[rvg])
            S.op("dve", _tt(vg, vg, ROW[:, 1, :], ALU.mult), reads=[rvg, r_ROW], writes=[rvg])
            S.op("dve", _tt(vn, vg, ROW[:, 2, :], ALU.add), reads=[rvg, r_ROW], writes=[rvn])
            for g in range(8):
                bk = s * 2 + g // 4
                S.op("pe", _mm(B.bank(bk)[:, (g % 4) * 128:(g % 4 + 1) * 128], vn[:, g * 128:(g + 1) * 128],
                               WST[:, g, :], True, True),
                     reads=[rvn, r_WS], writes=[B.bank_res[bk]])
            for gh in range(2):
                bk = s * 2 + gh
                tm, rtm = next_tmp(C)
                S.op("dve", _tt(tm, B.bank(bk), ROW[:, 0, gh * 512:(gh + 1) * 512], ALU.add),
                     reads=[B.bank_res[bk], r_ROW], writes=[rtm])
                S.op("dve", _tt(YAST[:, gh * 4:(gh + 1) * 4, s * 128:(s + 1) * 128],
                                tm.rearrange("p (a b) -> p a b", a=4),
                                U[:, gh * 4:(gh + 1) * 4, s * 128:(s + 1) * 128], ALU.mult),
                     reads=[rtm, r_U], writes=[r_YAST])
        S.op("sp", _dma(sc["yaT"][:, tsl].rearrange("(g p) t -> p g t", p=128), YAST), reads=[r_YAST],
             writes=[B.dram_res(sc["yaT"])], dma=True)

        def fm_to_scratch(col0, dst, scale, with_km=False):
            i = stg_i[0]; stg_i[0] = 1 - i
            stg, rstg = STG[i], r_STG[i]

            def ev(j):
                if with_km:
                    for hb in range(2):
                        S.op("act", (lambda e, j=j, t=t, hb=hb: e.activation(
                            stg[:, j, hb * 256:(hb + 1) * 256], B.bank(j)[:, hb * 256:(hb + 1) * 256], AF.Copy,
                            accum_out=KM[:, j, t * 2 + hb:t * 2 + hb + 1])),
                             reads=[B.bank_res[j]], writes=[rstg, r_KM])
                elif scale is not None:
                    S.op("dve", _ts(stg[:, j, :], B.bank(j), scale, None, ALU.mult), reads=[B.bank_res[j]], writes=[rstg])
                else:
                    S.op("act", _act(stg[:, j, :], B.bank(j), AF.Copy), reads=[B.bank_res[j]], writes=[rstg])
            fm_group(B, C, w_in, col0, 8, HN, r_HN, ev)
            S.op("sp", _dma(dst[:, tsl].rearrange("(g p) t -> p g t", p=128), stg), reads=[rstg],
                 writes=[B.dram_res(dst)], dma=True)
        if "qb" in SECT:
            fm_to_scratch(2048, sc["qbT"], qscale)
        if "kb" in SECT:
            fm_to_scratch(3072, sc["kbT"], None, with_km=True)
        if "vb" in SECT:
            tm_group(B, C, w_in, 4096, HN, r_HN)
        for s in (range(4) if "vb" in SECT else []):
            vb, rvb = VBST[s % 2], r_VBST[s % 2]
            S.op("act", _act(vb[:, 0:512], B.bank(s * 2), AF.Copy), reads=[B.bank_res[s * 2]], writes=[rvb])
            S.op("dve", _copy(vb[:, 512:1024], B.bank(s * 2 + 1)), reads=[B.bank_res[s * 2 + 1]], writes=[rvb])
            S.op("sp", _dma(sc["vb"][t * TT + s * 128:t * TT + (s + 1) * 128, :], vb), reads=[rvb],
                 writes=[B.dram_res(sc["vb"])], dma=True)
        if "qc" in SECT:
            fm_to_scratch(5120, sc["qcT"], 0.125)
        if "kcvc" not in SECT:
            continue
        i = stg_i[0]; stg_i[0] = 1 - i
        stg, rstg = STG[i], r_STG[i]

        def consume(k, view, sres_slot):
            S.op("pe", _mm(B.bank(0), view(0, 128), HN[:, k, :], k == 0, k == NCH - 1),
                 reads=[sres_slot, r_HN[k]], writes=[B.bank_res[0]])
            for s in range(4):
                S.op("pe", _mm(B.bank(1 + s)[:, 0:128], HN[:, k, s * 128:(s + 1) * 128], view(128, 128),
                               k == 0, k == NCH - 1),
                     reads=[sres_slot, r_HN[k]], writes=[B.bank_res[1 + s]])
        stream_weights(B, C, w_in, 0, NCH, [(6144, 256)], consume)
        S.op("act", _act(stg[:, 0, :], B.bank(0), AF.Copy), reads=[B.bank_res[0]], writes=[rstg])
        S.op("sp", _dma(sc["kcT"][:, tsl], stg[:, 0, :]), reads=[rstg], writes=[B.dram_res(sc["kcT"])], dma=True)
        for s in range(4):
            S.op("dve", _copy(VCST[:, s, :], B.bank(1 + s)[:, 0:128]), reads=[B.bank_res[1 + s]], writes=[r_VCST])
        S.op("sp", _dma(sc["vc"][tsl, :].rearrange("(s p) d -> p s d", p=128), VCST), reads=[r_VCST],
             writes=[B.dram_res(sc["vc"])], dma=True)
    kmf = KM.rearrange("p a b -> p (a b)")
    S.op("dve", _ts(kmf, kmf, 1.0 / 256.0, None, ALU.mult), reads=[r_KM], writes=[r_KM])
    S.op("sp", _dma(sc["kmT"], kmf), reads=[r_KM], writes=[B.dram_res(sc["kmT"])], dma=True)
    B.top = base


def mixB_stage(B, C, l, bconst, fconst, sc, do_moba=True, do_swa=True, NH=8, NQT=4):
    S = B.S
    S.barrier()
    base = B.top
    BC = B.alloc(NBC * 2, BF16, (NBC,)); r_BC = Res("bconst")
    FC = B.alloc(NFC * 4, F32, (NFC,)); r_FC = Res("fconst")
    for c0 in range(0, NBC, 2048):
        c1 = min(NBC, c0 + 2048)
        S.op("sp", _dma(BC[:, c0:c1], bconst[:, c0:c1]), writes=[r_BC], dma=True)
    for c0 in range(0, NFC, 1024):
        c1 = min(NFC, c0 + 1024)
        S.op("sp", _dma(FC[:, c0:c1], fconst[:, c0:c1]), writes=[r_FC], dma=True)
    IDENT = BC[:, BC_IDENT:BC_IDENT + 128]
    PT = [B.alloc(1024, BF16, (512,)) for _ in range(3)]
    r_PT = [Res(f"PT{i}") for i in range(3)]
    YB = B.alloc(4096, BF16, (2048,)); r_YB = Res("YB")
    QT = B.alloc(4096, BF16, (2048,)); r_QT = Res("QT")

    if do_moba:
        KT = B.alloc(16384, BF16, (4, 2048)); r_KT = Res("KT")
        V = B.alloc(16384, BF16, (64, 128)); r_V = Res("V")
        KTo = B.alloc(4096, BF16, (2048,)); r_KTo = Res("KTo")
        Vo = B.alloc(4096, BF16, (16, 128)); r_Vo = Res("Vo")
        KM = B.alloc(64, BF16, (4, 8)); r_KMh = Res("KMh")
        KMF = B.alloc(128, F32, (4, 8)); r_KMF = Res("KMF")
        SELBT = B.alloc(4096, BF16, (2048,)); r_SELBT = Res("SELBT")
        S.op("dve", lambda e: e.memset(SELBT, 0.0), writes=[r_SELBT])
        G = B.alloc(128, F32, (32,)); T8 = B.alloc(32, F32, (8,)); SELB = B.alloc(64, BF16, (32,))
        r_G = Res("G")
        for h in range(NH):
            hs = slice(h * 128, (h + 1) * 128)
            S.op("sp", _dma(KT, sc["kbT_all"].rearrange("(r f) t -> f r t", r=4)[hs, :, :]),
                 reads=[B.dram_res(sc["kbT_all"])], writes=[r_KT], dma=True)
            for r4 in range(4):
                S.op("sp", _dma(V[:, r4 * 16:(r4 + 1) * 16, :],
                                sc["vb_all"][r4 * 2048:(r4 + 1) * 2048, hs].rearrange("(c p) d -> p c d", p=128)),
                     reads=[B.dram_res(sc["vb_all"])], writes=[r_V], dma=True)
            S.op("sp", _dma(KTo, sc["kbT"][hs, :]), reads=[B.dram_res(sc["kbT"])], writes=[r_KTo], dma=True)
            S.op("sp", _dma(Vo, sc["vb"][:, hs].rearrange("(c p) d -> p c d", p=128)),
                 reads=[B.dram_res(sc["vb"])], writes=[r_Vo], dma=True)
            S.op("sp", _dma(QT, sc["qbT"][hs, :]), reads=[B.dram_res(sc["qbT"])], writes=[r_QT], dma=True)
            S.op("sp", _dma(KMF, sc["kmT_all"].rearrange("(r p) c -> p r c", p=128)[:, :, h * 8:(h + 1) * 8]),
                 reads=[B.dram_res(sc["kmT_all"])], writes=[r_KMF], dma=True)
            S.op("dve", _copy(KM, KMF), reads=[r_KMF], writes=[r_KMh])
            for qs in range(4 * NQT):
                gb, gres = B.bank(7), B.bank_res[7]
                S.op("pe", _mm(gb[:, 0:32], QT[:, qs * 128:(qs + 1) * 128], KM.rearrange("p a b -> p (a b)"), True, True),
                     reads=[r_QT, r_KMh], writes=[gres])
                S.op("dve", _tt(G, gb[:, 0:32], FC[:, FC_PASTB + qs * 32:FC_PASTB + (qs + 1) * 32], ALU.add),
                     reads=[gres, r_FC], writes=[r_G])
                S.op("dve", lambda e: e.max(T8, G), reads=[r_G], writes=[r_G])
                S.op("dve", _ts(T8[:, 2:3], T8[:, 2:3], -1e29, None, ALU.max), reads=[r_G], writes=[r_G])
                S.op("dve", _ts(SELB, G, T8[:, 2:3], NEG, ALU.is_lt, ALU.mult), reads=[r_G], writes=[r_G])
                tb = B.bank(6, BF16)
                S.op("pe", (lambda e, tb=tb: e.transpose(tb[0:32, 0:128], SELB, IDENT)), reads=[r_G, r_BC],
                     writes=[B.bank_res[6]])
                S.op("act", _act(SELBT[0:32, qs * 128:(qs + 1) * 128], tb[0:32, 0:128], AF.Copy),
                     reads=[B.bank_res[6]], writes=[r_SELBT])
                if "dbg_g" in sc and h == 0:
                    S.op("sp", _dma(sc["dbg_g"][:, qs * 32:(qs + 1) * 32], G), reads=[r_G], writes=[B.dram_res(sc["dbg_g"])], dma=True)
                    S.op("sp", _dma(sc["dbg_t8"][:, qs * 8:(qs + 1) * 8], T8), reads=[r_G], writes=[B.dram_res(sc["dbg_t8"])], dma=True)
                    S.op("sp", _dma(sc["dbg_selb"][:, qs * 32:(qs + 1) * 32], SELB), reads=[r_G], writes=[B.dram_res(sc["dbg_selb"])], dma=True)
            if "dbg_g" in sc and h == 0:
                S.op("sp", _dma(sc["dbg_selbt"], SELBT), reads=[r_SELBT], writes=[B.dram_res(sc["dbg_selbt"])], dma=True)
            for qt in range(NQT):
                qsl = slice(qt * 512, (qt + 1) * 512)
                chunks = []
                for kc in range(48 + 4 * (qt + 1)):
                    chunks.append(("f", KT[:, kc // 16, (kc % 16) * 128:(kc % 16 + 1) * 128], V[:, kc, :], kc // 2,
                                   [r_KT], [r_V], kc))
                for d4 in range(4):
                    ci = qt * 4 + d4
                    chunks.append(("d", KTo[:, ci * 128:(ci + 1) * 128], Vo[:, ci, :], d4, [r_KTo], [r_Vo], 64 + d4))
                nchk = len(chunks)
                bo, bd = 3, 4

                def qk(i):
                    kind, kap, vap, idx, rk, rv, acol = chunks[i]
                    bs = i % 3
                    S.op("pe", _mm(B.bank(bs), kap, QT[:, qsl], True, False), reads=rk + [r_QT], writes=[B.bank_res[bs]])
                    if kind == "f":
                        S.op("pe", _mm(B.bank(bs), BC[:, BC_EALL + idx * 128:BC_EALL + (idx + 1) * 128],
                                       SELBT[:, qsl], False, True),
                             reads=[r_BC, r_SELBT], writes=[B.bank_res[bs]])
                    else:
                        S.op("pe", _mm(B.bank(bs), IDENT, BC[:, BC_CB + idx * 512:BC_CB + (idx + 1) * 512], False, True),
                             reads=[r_BC], writes=[B.bank_res[bs]])
                    col = FC_ALB + (h * 4 + qt) * 68 + acol
                    S.op("act", _act(PT[bs], B.bank(bs), AF.Exp, bias=FC[:, col:col + 1]),
                         reads=[B.bank_res[bs], r_FC], writes=[r_PT[bs]])

                def pv(i):
                    kind, kap, vap, idx, rk, rv, acol = chunks[i]
                    bs = i % 3
                    S.op("pe", _mm(B.bank(bo), vap, PT[bs], i == 0, i == nchk - 1), reads=rv + [r_PT[bs]],
                         writes=[B.bank_res[bo]])
                    S.op("pe", _mm(B.bank(bd), C.ones, PT[bs], i == 0, i == nchk - 1), reads=[r_PT[bs], C.r_const],
                         writes=[B.bank_res[bd]])
                qk(0)
                for i in range(nchk):
                    if i + 1 < nchk:
                        qk(i + 1)
                    pv(i)
                tm, rtm = next_tmp(C)
                S.op("dve", lambda e, tm=tm, bd=bd: e.reciprocal(tm, B.bank(bd)), reads=[B.bank_res[bd]], writes=[rtm])
                S.op("dve", _tt(YB[:, qsl], B.bank(bo), tm, ALU.mult), reads=[B.bank_res[bo], rtm], writes=[r_YB])
            S.op("sp", _dma(sc["ybT"][hs, :], YB), reads=[r_YB], writes=[B.dram_res(sc["ybT"])], dma=True)

    if do_swa:
        KCT = B.alloc(4352, BF16, (2176,)); r_KCT = Res("KCT")
        VCt = B.alloc(4352, BF16, (17, 128)); r_VCt = Res("VCt")
        VPA = B.alloc(4352, BF16, (17, 128)); VPB = B.alloc(4352, BF16, (17, 128)); r_VP = Res("VP")
        HK = B.alloc(1024, BF16, (4, 128)); HV = B.alloc(1024, BF16, (4, 128)); r_H = Res("halo")
        SINKE = B.alloc(32, F32, (8,)); r_SK = Res("sinke")
        TS = B.alloc(2048, F32, (512,)); r_TS = Res("TS")
        S.op("sp", _dma(KCT[:, 128:2176], sc["kcT"]), reads=[B.dram_res(sc["kcT"])], writes=[r_KCT], dma=True)
        S.op("sp", _dma(VCt[:, 1:17, :], sc["vc"].rearrange("(c p) d -> p c d", p=128)),
             reads=[B.dram_res(sc["vc"])], writes=[r_VCt], dma=True)
        S.op("sp", _dma(HK, sc["kcT_all"].rearrange("(r f) t -> f r t", r=4)[:, :, 1920:2048]),
             reads=[B.dram_res(sc["kcT_all"])], writes=[r_H], dma=True)
        S.op("sp", _dma(HV, sc["vc_all"].rearrange("(r c p) d -> p r c d", r=4, p=128)[:, :, 15, :]),
             reads=[B.dram_res(sc["vc_all"])], writes=[r_H], dma=True)
        for (dst, src, rd) in ((KCT[:, 0:128], HK, r_KCT), (VCt[:, 0, :], HV, r_VCt)):
            S.op("dve", _ts(dst, src[:, 0, :], FC[:, FC_HALOW:FC_HALOW + 1], None, ALU.mult), reads=[r_H, r_FC], writes=[rd])
            for r4 in range(1, 4):
                S.op("dve", _stt(dst, src[:, r4, :], FC[:, FC_HALOW + r4:FC_HALOW + r4 + 1], dst, ALU.mult, ALU.add),
                     reads=[r_H, r_FC, rd], writes=[rd])
        SWA_STOP = int(os.environ.get("SWA_STOP", "9"))
        KA = B.alloc(4352, BF16, (2176,)); KB = B.alloc(4352, BF16, (2176,)); r_KAB = Res("KAB")
        S.op("dve", lambda e: e.memset(KA, 0.0), writes=[r_KAB])
        S.op("dve", lambda e: e.memset(KB, 0.0), writes=[r_KAB])
        S.op("dve", _copy(KA[0:64, :], KCT[0:64, :]), reads=[r_KCT], writes=[r_KAB])
        S.op("dve", _copy(KB[64:128, :], KCT[64:128, :]), reads=[r_KCT], writes=[r_KAB])
        S.op("dve", lambda e: e.memset(VPA, 0.0), writes=[r_VP])
        S.op("dve", lambda e: e.memset(VPB, 0.0), writes=[r_VP])
        S.op("dve", _copy(VPA[:, :, 0:64], VCt[:, :, 0:64]), reads=[r_VCt], writes=[r_VP])
        S.op("dve", _copy(VPB[:, :, 64:128], VCt[:, :, 64:128]), reads=[r_VCt], writes=[r_VP])
        S.op("act", _act(SINKE, C.gains[:, SINK0 + l * 8:SINK0 + (l + 1) * 8], AF.Exp), reads=[C.r_gains], writes=[r_SK])
        ONEA = BC[:, BC_ONEA:BC_ONEA + 128]
        ONEB = BC[:, BC_ONEB:BC_ONEB + 128]
        for j in range(8 if SWA_STOP >= 2 else 0):
            S.op("sp", _dma(QT, sc["qcT"][j * 128:(j + 1) * 128, :]), reads=[B.dram_res(sc["qcT"])], writes=[r_QT], dma=True)
            for b in range(4 * NQT if SWA_STOP >= 3 else 0):
                bs = b % 2
                sb, sres = B.bank(bs), B.bank_res[bs]
                bo, bd = 2 + (b // 4) % 2, 4 + (b // 4) % 2
                for hf in range(2):
                    psl = slice(hf * 64, (hf + 1) * 64)
                    for w in range(2):
                        S.op("pe", _mm(sb[:, (hf * 2 + w) * 128:(hf * 2 + w + 1) * 128],
                                       (KA if hf == 0 else KB)[:, (b + w) * 128:(b + w + 1) * 128],
                                       QT[:, b * 128:(b + 1) * 128], True, True),
                             reads=[r_KAB, r_QT], writes=[sres])
                var = 1 if b == 0 else 0
                col = FC_SWAB + (j * 2 + var) * 512
                S.op("dve", _tt(TS, sb, FC[:, col:col + 512], ALU.add), reads=[sres, r_FC], writes=[r_TS])
                pt, rpt = PT[b % 3], r_PT[b % 3]
                S.op("act", _act(pt, TS, AF.Exp), reads=[r_TS], writes=[rpt])
                if SWA_STOP < 4:
                    continue
                osl = slice((b % 4) * 128, (b % 4 + 1) * 128)
                mats = [(VPA[:, b, :], ONEA, 0), (VPA[:, b + 1, :], ONEA, 1), (VPB[:, b, :], ONEB, 2), (VPB[:, b + 1, :], ONEB, 3)]
                for i, (vp, on, c4) in enumerate(mats):
                    S.op("pe", _mm(B.bank(bo)[:, osl], vp, pt[:, c4 * 128:(c4 + 1) * 128], i == 0, i == 3),
                         reads=[r_VP, rpt], writes=[B.bank_res[bo]])
                for i, (vp, on, c4) in enumerate(mats):
                    S.op("pe", _mm(B.bank(bd)[:, osl], on, pt[:, c4 * 128:(c4 + 1) * 128], i == 0, i == 3),
                         reads=[r_BC, rpt], writes=[B.bank_res[bd]])
                if b % 4 == 3:
                    tm, rtm = next_tmp(C)
                    S.op("dve", _ts(tm, B.bank(bd), C_sink(SINKE, j), None, ALU.add), reads=[B.bank_res[bd], r_SK], writes=[rtm])
                    S.op("dve", lambda e, tm=tm: e.reciprocal(tm, tm), reads=[rtm], writes=[rtm])
                    S.op("dve", _tt(YB[:, (b // 4) * 512:(b // 4 + 1) * 512], B.bank(bo), tm, ALU.mult),
                         reads=[B.bank_res[bo], rtm], writes=[r_YB])
            S.op("sp", _dma(sc["ycT"][j * 128:(j + 1) * 128, :], YB), reads=[r_YB], writes=[B.dram_res(sc["ycT"])], dma=True)
    B.top = base


def C_sink(SINKE, j):
    return SINKE[:, j:j + 1]


def mixC_stage(B, C, l, x_in, x_out, yscr, w_gate, w_branch, w_out, sc):
    S = B.S
    S.barrier()
    base = B.top
    X = B.alloc(65536, F32, (32, 512))
    r_X = [Res(f"X{q}") for q in range(4)]
    HN = B.alloc(32768, BF16, (32, 512))
    r_HN = [Res(f"HN{c}") for c in range(NCH)]
    YT = [B.alloc(8192, BF16, (8, 512)) for _ in range(3)]
    r_YT = [Res(f"YT{n}") for n in range(3)]
    SGT = B.alloc(8192, F32, (4, 512)); r_SGT = [Res(f"SGT{j}") for j in range(4)]
    ACC = B.alloc(8192, F32, (4, 512)); r_ACC = [Res(f"ACC{j}") for j in range(4)]
    MERGED = X.rearrange("p a b -> p (a b)")[:, 0:8192].bitcast(BF16).rearrange("p (a b) -> p a b", a=32)
    r_M = [Res(f"M{c}") for c in range(NCH)]
    for c in range(NCH):
        for q in range(4):
            alias(r_M[c], r_X[q])
    ysrc = [sc["yaT"], sc["ybT"], sc["ycT"]]
    B.cache_res = Res("wcache"); B.caches = {}; B.uid += 1
    for t in range(B.NT):
        B.cur_t = t
        tsl = slice(t * TT, (t + 1) * TT)
        if t == 0:
            load_x_tile(B, C, X, r_X, x_in, t)
        make_hn(B, C, X, r_X, HN, r_HN, gcol(l, 2))
        for n in range(3):
            S.op("sp", _dma(YT[n], ysrc[n][:, tsl].rearrange("(k p) t -> p k t", p=128)),
                 reads=[B.dram_res(ysrc[n])], writes=[r_YT[n]], dma=True)
        for mg in range(8):
            for n in range(3):
                def consume_g(k, view, sres_slot):
                    for j in range(4):
                        S.op("pe", _mm(B.bank(j), view(j * 128, 128), HN[:, k, :], k == 0, k == NCH - 1),
                             reads=[sres_slot, r_HN[k]], writes=[B.bank_res[j]])
                stream_weights(B, C, w_gate, 0, NCH, [(n * D + mg * 512, 512)], consume_g)

                def consume_b(k, view, sres_slot, n=n):
                    for j in range(4):
                        S.op("pe", _mm(B.bank(4 + j), view(j * 128, 128), YT[n][:, k, :], k == 0, k == 7),
                             reads=[sres_slot, r_YT[n]], writes=[B.bank_res[4 + j]])
                stream_weights(B, C, w_branch, n * BW, 8, [(mg * 512, 512)], consume_b)
                for j in range(4):
                    m = mg * 4 + j
                    bcol = BG0 + l * 96 + n * 32 + m
                    S.op("act", _act(SGT[:, j, :], B.bank(j), AF.Sigmoid, bias=C.gains[:, bcol:bcol + 1]),
                         reads=[B.bank_res[j], C.r_gains], writes=[r_SGT[j]])
                    if n == 0:
                        S.op("dve", _tt(ACC[:, j, :], SGT[:, j, :], B.bank(4 + j), ALU.mult),
                             reads=[r_SGT[j], B.bank_res[4 + j]], writes=[r_ACC[j]])
                    else:
                        S.op("dve", _tt(SGT[:, j, :], SGT[:, j, :], B.bank(4 + j), ALU.mult),
                             reads=[r_SGT[j], B.bank_res[4 + j]], writes=[r_SGT[j]])
                        if n == 1:
                            S.op("dve", _tt(ACC[:, j, :], ACC[:, j, :], SGT[:, j, :], ALU.add),
                                 reads=[r_SGT[j], r_ACC[j]], writes=[r_ACC[j]])
                        else:
                            S.op("dve", _tt(MERGED[:, m, :], ACC[:, j, :], SGT[:, j, :], ALU.add),
                                 reads=[r_SGT[j], r_ACC[j]], writes=[r_M[m]])
        out_groups(B, C, w_out, 0, [MERGED[:, k, :] for k in range(NCH)], r_M, yscr)
        if t + 1 < B.NT:
            load_x_tile(B, C, X, r_X, x_in, t + 1)
        residual_tail(B, C, t, x_in, x_out, yscr, gcol(l, 3), False)
    B.top = base


def _slopes():
    i = np.arange(1, 25, dtype=np.float32)
    s = np.exp2(-8.0 * i / 24.0).astype(np.float32)
    return s[:16], s[16:]


_QC_PERM = np.concatenate([np.concatenate([j * 64 + np.arange(64), (8 + j) * 64 + np.arange(64)]) for j in range(8)])


def make_bconst():
    bc = np.zeros((128, NBC), np.float32)
    bc[:, BC_IDENT:BC_IDENT + 128] = np.eye(128)
    p = np.arange(128)
    bc[:, BC_TRI:BC_TRI + 128] = (p[:, None] <= p[None, :])
    for n in range(32):
        bc[n, BC_EALL + n * 128:BC_EALL + (n + 1) * 128] = 1.0
    q = np.arange(512)
    for d4 in range(4):
        ok = ((d4 // 2) == (q[None, :] // 256)) & ((d4 * 128 + p[:, None]) <= q[None, :])
        bc[:, BC_CB + d4 * 512:BC_CB + (d4 + 1) * 512] = np.where(ok, 0.0, NEG)
    bc[:, BC_ONEA:BC_ONEA + 64] = 1.0
    bc[:, BC_ONEB + 64:BC_ONEB + 128] = 1.0
    return bc.astype(ml_dtypes.bfloat16)


def make_fconst(r):
    fc = np.zeros((128, NFC), np.float32)
    swa_s, moba_s = _slopes()
    p = np.arange(128, dtype=np.float32)
    for h in range(8):
        for qt in range(4):
            q0 = r * 2048 + qt * 512
            c0 = FC_ALB + (h * 4 + qt) * 68
            for kc in range(64):
                fc[:, c0 + kc] = moba_s[h] * (kc * 128 + p - q0)
            for d4 in range(4):
                fc[:, c0 + 64 + d4] = moba_s[h] * (d4 * 128 + p)
    for qs in range(16):
        blk = r * 8 + qs // 2
        n = np.arange(32)
        fc[:, FC_PASTB + qs * 32:FC_PASTB + (qs + 1) * 32] = np.where(n < blk, 0.0, -1e30)[None, :]
    s_loc = np.arange(128)[:, None]
    t_loc = np.arange(128)[None, :]
    d_cur = (t_loc - s_loc).astype(np.float32)
    d_prev = (t_loc + 128 - s_loc).astype(np.float32)
    for j in range(8):
        for var in range(2):
            c0 = FC_SWAB + (j * 2 + var) * 512
            for hf, h in enumerate((j, 8 + j)):
                prev = np.where(d_prev < 128, -swa_s[h] * d_prev, -1e30)
                if var == 1 and r == 0:
                    prev = np.full((128, 128), -1e30, np.float32)
                cur = np.where(d_cur >= 0, -swa_s[h] * d_cur, -1e30)
                fc[:, c0 + (hf * 2) * 128:c0 + (hf * 2 + 1) * 128] = prev
                fc[:, c0 + (hf * 2 + 1) * 128:c0 + (hf * 2 + 2) * 128] = cur
    if r > 0:
        fc[:, FC_HALOW + r - 1] = 1.0
    return fc


def pvec(v):
    return np.ascontiguousarray(np.asarray(v, np.float32).reshape(-1, 128).T)


def make_gains(inp):
    g = np.zeros((128, NG), np.float32)
    names = ["ffn1_pre_g", "ffn1_post_g", "mix_pre_g", "mix_post_g", "ffn2_pre_g", "ffn2_post_g"]
    for l in range(2):
        for i, nm in enumerate(names):
            g[:, gcol(l, i):gcol(l, i) + 32] = pvec(inp[nm][l])
        g[:, BG0 + l * 96:BG0 + (l + 1) * 96] = pvec(inp["b_gate"][l])
        sk = np.asarray(inp["swa_sinks"][l], np.float32)
        g[:64, SINK0 + l * 8:SINK0 + (l + 1) * 8] = sk[None, 0:8]
        g[64:, SINK0 + l * 8:SINK0 + (l + 1) * 8] = sk[None, 8:16]
    return g


def layer_arrays(inp, l, skip_ffn=False):
    w_in = np.array(inp["w_in"][l], np.float32, copy=True)
    w_in[:, 5120:6144] = w_in[:, 5120 + _QC_PERM]
    wb = np.array(inp["w_branch"][l], np.float32, copy=True)
    wb[2] = wb[2][_QC_PERM, :]
    a = {} if skip_ffn else {
        f"ffn1_w_up_{l}": inp["ffn1_w_up"][l], f"ffn1_w_down_{l}": inp["ffn1_w_down"][l],
        f"ffn2_w_up_{l}": inp["ffn2_w_up"][l], f"ffn2_w_down_{l}": inp["ffn2_w_down"][l]}
    a.update({
        f"w_in_{l}": w_in, f"w_gate_{l}": inp["w_gate"][l], f"w_branch_{l}": wb.reshape(3 * BW, D),
        f"w_out_{l}": inp["w_out"][l],
        f"wsT_{l}": np.ascontiguousarray(np.asarray(inp["gmlp_w_s"][l], np.float32).transpose(2, 0, 1)),
        f"rowtab_{l}": np.ascontiguousarray(np.broadcast_to(np.stack([
            np.asarray(inp["gmlp_b_s"][l], np.float32).reshape(1024),
            np.asarray(inp["gmlp_ln_g"][l], np.float32), np.asarray(inp["gmlp_ln_b"][l], np.float32)])[None], (128, 3, 1024))),
    })
    return {k: np.ascontiguousarray(np.asarray(v, np.float32)) for k, v in a.items()}


SCRATCH = {
    "qbT": ([BW, TOK], BF16), "kbT": ([BW, TOK], BF16), "vb": ([TOK, BW], BF16), "kmT": ([128, 64], F32),
    "qcT": ([BW, TOK], BF16), "kcT": ([128, TOK], BF16), "vc": ([TOK, 128], BF16), "yaT": ([BW, TOK], BF16),
    "ybT": ([BW, TOK], BF16), "ycT": ([BW, TOK], BF16),
    "kbT_all": ([4 * BW, TOK], BF16), "vb_all": ([4 * TOK, BW], BF16), "kmT_all": ([512, 64], F32),
    "kcT_all": ([4 * 128, TOK], BF16), "vc_all": ([4 * TOK, 128], BF16),
}
WSHAPES = {"ffn1_w_up": [D, 2 * DFF], "ffn1_w_down": [DFF, D], "ffn2_w_up": [D, 2 * DFF], "ffn2_w_down": [DFF, D],
           "w_in": [D, IN_COLS], "w_gate": [D, 3 * D], "w_branch": [3 * BW, D], "w_out": [D, D],
           "wsT": [128, 8, 128], "rowtab": [128, 3, 1024]}


def build_launch(stages, ext_in, ext_out, NT=4, mixb_kw=None):
    def stage_fn(nc, B):
        made = {}

        def T(name, shape, dtype):
            if name not in made:
                kind = "ExternalInput" if name in ext_in else ("ExternalOutput" if name in ext_out else "Internal")
                made[name] = nc.dram_tensor(name, shape, dtype, kind=kind).ap()
            return made[name]

        def W(nm, l):
            return T(f"{nm}_{l}", WSHAPES[nm], F32)

        def SC(l):
            d = {k: T(f"{k}_{l}", sh, dt) for k, (sh, dt) in SCRATCH.items()}
            if os.environ.get("MIXB_DBG"):
                d["dbg_g"] = T("dbg_g", [128, 512], F32); d["dbg_t8"] = T("dbg_t8", [128, 128], F32)
                d["dbg_selb"] = T("dbg_selb", [128, 512], BF16); d["dbg_selbt"] = T("dbg_selbt", [128, 2048], BF16)
            return d
        bconst = T("bconst", [128, NBC], BF16)
        fconst = T("fconst", [128, NFC], F32)
        gains = T("gains", [128, NG], F32)
        C = setup_common(B, bconst, gains)
        yscr = T("yscr", [D, TT], F32)
        outs = []
        for st in stages:
            if st[0] == "ffn":
                _, l, which, xi, xo = st
                pre, post = (0, 1) if which == 1 else (4, 5)
                ffn_stage(B, C, T(xi, [D, TOK], F32), T(xo, [D, TOK], F32), yscr,
                          W(f"ffn{which}_w_up", l), W(f"ffn{which}_w_down", l), gcol(l, pre), gcol(l, post))
            elif st[0] == "mixA":
                _, l, xi = st
                mixA_stage(B, C, l, T(xi, [D, TOK], F32), W("w_in", l), W("wsT", l), W("rowtab", l), bconst, SC(l))
            elif st[0] == "mixB":
                _, l = st
                mixB_stage(B, C, l, bconst, fconst, SC(l), **(mixb_kw or {}))
            elif st[0] == "mixC":
                _, l, xi, xo = st
                mixC_stage(B, C, l, T(xi, [D, TOK], F32), T(xo, [D, TOK], F32), yscr,
                           W("w_gate", l), W("w_branch", l), W("w_out", l), SC(l))
        final_wait(B, [B.dram_res(made[n]) for n in made if n in ext_out])
    return make_program(stage_fn, NT=NT)


OWN = ["qbT", "kbT", "vb", "kmT", "qcT", "kcT", "vc", "yaT"]
GATH = ["kbT", "vb", "kmT", "kcT", "vc"]


def run_launch(nc, in_maps, n_cores=8):
    res = run_bass_kernel_spmd(nc, in_maps, core_ids=list(range(n_cores)))
    return res.results


def gather_maps(outs, l, n_cores=8):
    g = []
    for c in range(n_cores):
        b0 = (c // 4) * 4
        d = {}
        for k in GATH:
            d[f"{k}_all_{l}"] = np.concatenate([outs[b0 + r][f"{k}_{l}"] for r in range(4)], axis=0)
        g.append(d)
    return g


def kernel(**inp):
    inp = {k: np.asarray(v) for k, v in inp.items()}
    n = 8
    bconst = make_bconst()
    gains = make_gains(inp)
    fcs = [make_fconst(c % 4) for c in range(n)]
    LA = [layer_arrays(inp, 0), layer_arrays(inp, 1)]
    x = inp["x"]
    base = [{"bconst": bconst, "gains": gains, "fconst": fcs[c]} for c in range(n)]
    consts = {"bconst", "gains", "fconst"}
    own0 = {f"{k}_0" for k in OWN}
    own1 = {f"{k}_1" for k in OWN}
    all0 = {f"{k}_all_0" for k in GATH}
    all1 = {f"{k}_all_1" for k in GATH}

    def wnames(l, names):
        return {f"{nm}_{l}" for nm in names}

    w1 = wnames(0, ["ffn1_w_up", "ffn1_w_down", "w_in", "wsT", "rowtab"])
    nc1 = build_launch([("ffn", 0, 1, "x0", "x1"), ("mixA", 0, "x1")], consts | {"x0"} | w1, {"x1"} | own0)
    maps = []
    for c in range(n):
        m = dict(base[c])
        m["x0"] = np.ascontiguousarray(x[c // 4, (c % 4) * TOK:(c % 4 + 1) * TOK, :].T)
        m.update({k: LA[0][k] for k in w1})
        maps.append(m)
    o1 = run_launch(nc1, maps)
    del maps
    w2 = wnames(0, ["w_gate", "w_branch", "w_out", "ffn2_w_up", "ffn2_w_down"]) | \
        wnames(1, ["ffn1_w_up", "ffn1_w_down", "w_in", "wsT", "rowtab"])
    nc2 = build_launch([("mixB", 0), ("mixC", 0, "x1", "x2"), ("ffn", 0, 2, "x2", "x3"), ("ffn", 1, 1, "x3", "x4"),
                        ("mixA", 1, "x4")], consts | {"x1"} | own0 | all0 | w2, {"x4"} | own1)
    g1 = gather_maps(o1, 0)
    maps = []
    for c in range(n):
        m = dict(base[c])
        m["x1"] = o1[c]["x1"]
        m.update({k: o1[c][k] for k in own0})
        m.update(g1[c])
        for k in w2:
            m[k] = LA[int(k[-1])][k]
        maps.append(m)
    o2 = run_launch(nc2, maps)
    del maps, o1, g1
    w3 = wnames(1, ["w_gate", "w_branch", "w_out", "ffn2_w_up", "ffn2_w_down"])
    nc3 = build_launch([("mixB", 1), ("mixC", 1, "x4", "x5"), ("ffn", 1, 2, "x5", "xout")],
                       consts | {"x4"} | own1 | all1 | w3, {"xout"})
    g2 = gather_maps(o2, 1)
    maps = []
    for c in range(n):
        m = dict(base[c])
        m["x4"] = o2[c]["x4"]
        m.update({k: o2[c][k] for k in own1})
        m.update(g2[c])
        m.update({k: LA[1][k] for k in w3})
        maps.append(m)
    o3 = run_launch(nc3, maps)
    out = np.empty_like(x)
    for c in range(n):
        out[c // 4, (c % 4) * TOK:(c % 4 + 1) * TOK, :] = o3[c]["xout"].T
    return out
```
